# Optimizing a Trainium2 kernel written in Bass

```python
import jax
import jax.numpy as jnp
from jax import lax
import numpy as np

D_MODEL = 1024
BATCH = 4
SEQ = 8192
DEPTH = 1

MIX_WIDTH = D_MODEL
HGRN_WIDTH = MIX_WIDTH // 2
HGRN_HEAD_DIM = 128
HGRN_HEADS = HGRN_WIDTH // HGRN_HEAD_DIM
HGRN_CHUNK = 64
GMLP_WIDTH = MIX_WIDTH - HGRN_WIDTH
GMLP_GROUP_DIM = 128
GMLP_GROUPS = GMLP_WIDTH // GMLP_GROUP_DIM
GMLP_CHUNK = 128
IN_COLS = 4 * HGRN_WIDTH + 2 * GMLP_WIDTH
PEER_HEADS = 8
PEER_N_KEYS = 128
PEER_N_EXPERTS = PEER_N_KEYS * PEER_N_KEYS
PEER_TOPK = 16
PEER_QUERY_DIM = 256
PEER_HALF = PEER_QUERY_DIM // 2
PEER_TOKEN_BLOCK = 128
NORM_EPS = 1e-6

kernel_name = 'hymba_hgrn2_gmlp_peer_adaln_layer'


def rmsnorm(x, g):
    xf = x.astype(jnp.float32)
    y = xf * lax.rsqrt(jnp.mean(xf * xf, axis=-1, keepdims=True) + NORM_EPS)
    return (y * g.astype(jnp.float32)).astype(x.dtype)


def layernorm(x, g, b):
    xf = x.astype(jnp.float32)
    mu = jnp.mean(xf, axis=-1, keepdims=True)
    var = jnp.mean(jnp.square(xf - mu), axis=-1, keepdims=True)
    y = (xf - mu) * lax.rsqrt(var + NORM_EPS)
    return (y * g.astype(jnp.float32) + b.astype(jnp.float32)).astype(x.dtype)


def hgrn2_chunkwise(q, k, v, logf):
    b_, s_, h_, dk = q.shape
    dv = v.shape[-1]
    nc = s_ // HGRN_CHUNK

    def to_chunks(t):
        return t.reshape(b_, nc, HGRN_CHUNK, h_, t.shape[-1]).transpose(1, 0, 3, 2, 4)

    qc, kc, vc = to_chunks(q), to_chunks(k), to_chunks(v)
    bc = jnp.cumsum(to_chunks(logf), axis=-2)
    causal = jnp.tril(jnp.ones((HGRN_CHUNK, HGRN_CHUNK), dtype=bool))[:, :, None]

    def step(state, inp):
        q_c, k_c, v_c, b_c = inp
        diff = b_c[..., :, None, :] - b_c[..., None, :, :]
        decay = jnp.exp(jnp.where(causal, diff, -jnp.inf))
        scores = jnp.einsum('bhtk,bhtsk,bhsk->bhts', q_c, decay, k_c)
        intra = jnp.einsum('bhts,bhsv->bhtv', scores, v_c)
        inter = jnp.einsum('bhtk,bhkv->bhtv', q_c * jnp.exp(b_c), state)
        b_last = b_c[..., -1:, :]
        new_state = (jnp.exp(b_last[..., 0, :])[..., None] * state
                     + jnp.einsum('bhsk,bhsv->bhkv', k_c * jnp.exp(b_last - b_c), v_c))
        return new_state, intra + inter

    state0 = jnp.zeros((b_, h_, dk, dv), jnp.float32)
    _, oc = lax.scan(step, state0, (qc, kc, vc, bc))
    return oc.transpose(1, 0, 3, 2, 4).reshape(b_, s_, h_, dv)


def hybrid_mixer(h, w_in, lb, hgrn_norm_g, gmlp_ln_g, gmlp_ln_b, spatial_w, spatial_b, gmlp_norm_g, w_out):
    b_, s_, _ = h.shape
    f32 = jnp.float32
    proj = h @ w_in
    cuts = [HGRN_WIDTH, 2 * HGRN_WIDTH, 3 * HGRN_WIDTH, 4 * HGRN_WIDTH, 4 * HGRN_WIDTH + GMLP_WIDTH]
    q, fz, inp, og, u, v = jnp.split(proj, cuts, axis=-1)

    heads = lambda t: t.reshape(b_, s_, HGRN_HEADS, HGRN_HEAD_DIM)
    fz32 = fz.astype(f32)
    logf = jnp.log(lb + (1.0 - lb) * jax.nn.sigmoid(fz32))
    key_gate = (1.0 - lb) * jax.nn.sigmoid(-fz32)
    q_feat = jax.nn.silu(q.astype(f32))
    o = hgrn2_chunkwise(heads(q_feat), heads(key_gate), heads(inp.astype(f32)), heads(logf))
    o = rmsnorm(o, hgrn_norm_g.reshape(HGRN_HEADS, HGRN_HEAD_DIM))
    o = o * heads(jax.nn.silu(og.astype(f32)))
    o = o.reshape(b_, s_, HGRN_WIDTH).astype(h.dtype)

    u = jax.nn.gelu(u)
    v = layernorm(jax.nn.gelu(v), gmlp_ln_g, gmlp_ln_b)
    nch = s_ // GMLP_CHUNK
    vc = v.reshape(b_, nch, GMLP_CHUNK, GMLP_GROUPS, GMLP_GROUP_DIM)
    tri = jnp.tril(jnp.ones((GMLP_CHUNK, GMLP_CHUNK), dtype=bool))[None]
    w_mask = jnp.where(tri, spatial_w, 0)
    mixed = jnp.einsum('gts,bnsgc->bntgc', w_mask, vc) + spatial_b.T[None, None, :, :, None]
    gm = u.reshape(b_, nch, GMLP_CHUNK, GMLP_GROUPS, GMLP_GROUP_DIM) * mixed
    gm = rmsnorm(gm.reshape(b_, s_, GMLP_GROUPS, GMLP_GROUP_DIM), gmlp_norm_g.reshape(GMLP_GROUPS, GMLP_GROUP_DIM))
    gm = gm.reshape(b_, s_, GMLP_WIDTH).astype(h.dtype)

    return jnp.concatenate([o, gm], axis=-1) @ w_out


def peer_ffn(h, wq, keys, u_tab, v_tab):
    b_, s_, d_ = h.shape
    t_ = b_ * s_
    hf = h.reshape(t_, d_)
    q = (hf @ wq).reshape(t_, PEER_HEADS, 2, PEER_HALF)
    sim = jnp.einsum('thpd,hpnd->thpn', q, keys)
    s1, i1 = lax.top_k(sim[:, :, 0, :], PEER_TOPK)
    s2, i2 = lax.top_k(sim[:, :, 1, :], PEER_TOPK)
    cand_s = (s1[..., :, None] + s2[..., None, :]).reshape(t_, PEER_HEADS, PEER_TOPK * PEER_TOPK)
    cand_i = (i1[..., :, None] * PEER_N_KEYS + i2[..., None, :]).reshape(t_, PEER_HEADS, PEER_TOPK * PEER_TOPK)
    top_s, pos = lax.top_k(cand_s, PEER_TOPK)
    idx = jnp.take_along_axis(cand_i, pos, axis=-1)
    gate = jax.nn.softmax(top_s.astype(jnp.float32), axis=-1).astype(h.dtype)
    nb = t_ // PEER_TOKEN_BLOCK
    idx_b = idx.reshape(nb, PEER_TOKEN_BLOCK, PEER_HEADS * PEER_TOPK)
    gate_b = gate.reshape(nb, PEER_TOKEN_BLOCK, PEER_HEADS * PEER_TOPK)
    h_b = hf.reshape(nb, PEER_TOKEN_BLOCK, d_)

    def block(args):
        hb, ib, gb = args
        u_sel = jnp.take(u_tab, ib, axis=0)
        act = jax.nn.gelu(jnp.einsum('tkd,td->tk', u_sel, hb)) * gb
        return jnp.einsum('tk,tkd->td', act, jnp.take(v_tab, ib, axis=0))

    y = lax.map(block, (h_b, idx_b, gate_b))
    return y.reshape(b_, s_, d_)


def setup_inputs(seed: int = 0) -> dict:
    key = jax.random.key(seed)
    ks = jax.random.split(key, 20)
    f32 = jnp.float32

    def nrm(k, shape, scale):
        return jax.random.normal(k, shape, f32) * scale

    return {
        'x': nrm(ks[0], (BATCH, SEQ, D_MODEL), 1.0),
        'c': nrm(ks[1], (BATCH, D_MODEL), 1.0),
        'ada_w': nrm(ks[2], (DEPTH, D_MODEL, 6 * D_MODEL), 0.5 * D_MODEL ** -0.5),
        'ada_b': nrm(ks[3], (DEPTH, 6 * D_MODEL), 0.01),
        'norm1_g': 1.0 + nrm(ks[4], (DEPTH, D_MODEL), 0.02),
        'w_in': nrm(ks[5], (DEPTH, D_MODEL, IN_COLS), D_MODEL ** -0.5),
        'lb_gamma': nrm(ks[6], (DEPTH + 1, HGRN_WIDTH), 0.1),
        'hgrn_norm_g': 1.0 + nrm(ks[7], (DEPTH, HGRN_WIDTH), 0.02),
        'gmlp_ln_g': 1.0 + nrm(ks[8], (DEPTH, GMLP_WIDTH), 0.02),
        'gmlp_ln_b': nrm(ks[9], (DEPTH, GMLP_WIDTH), 0.02),
        'spatial_w': nrm(ks[10], (DEPTH, GMLP_GROUPS, GMLP_CHUNK, GMLP_CHUNK), GMLP_CHUNK ** -0.5),
        'spatial_b': 1.0 + nrm(ks[11], (DEPTH, GMLP_GROUPS, GMLP_CHUNK), 0.02),
        'gmlp_norm_g': 1.0 + nrm(ks[12], (DEPTH, GMLP_WIDTH), 0.02),
        'w_out': nrm(ks[13], (DEPTH, MIX_WIDTH, D_MODEL), MIX_WIDTH ** -0.5),
        'norm2_g': 1.0 + nrm(ks[14], (DEPTH, D_MODEL), 0.02),
        'peer_wq': nrm(ks[15], (DEPTH, D_MODEL, PEER_HEADS * PEER_QUERY_DIM), D_MODEL ** -0.5),
        'peer_keys': nrm(ks[16], (DEPTH, PEER_HEADS, 2, PEER_N_KEYS, PEER_HALF), PEER_HALF ** -0.5),
        'peer_u': nrm(ks[17], (DEPTH, PEER_N_EXPERTS, D_MODEL), D_MODEL ** -0.5),
        'peer_v': nrm(ks[18], (DEPTH, PEER_N_EXPERTS, D_MODEL), 0.5),
        'final_g': 1.0 + nrm(ks[19], (D_MODEL,), 0.02),
    }


def reference(x, c, ada_w, ada_b, norm1_g, w_in, lb_gamma, hgrn_norm_g, gmlp_ln_g, gmlp_ln_b,
              spatial_w, spatial_b, gmlp_norm_g, w_out, norm2_g, peer_wq, peer_keys, peer_u, peer_v, final_g):
    lower_bounds = jnp.cumsum(jax.nn.softmax(lb_gamma.astype(jnp.float32), axis=0), axis=0)
    c_act = jax.nn.silu(c)
    for l in range(DEPTH):
        mod = c_act @ ada_w[l] + ada_b[l]
        shift1, scale1, gate1, shift2, scale2, gate2 = [m[:, None, :] for m in jnp.split(mod, 6, axis=-1)]
        h = rmsnorm(x, norm1_g[l]) * (1 + scale1) + shift1
        x = x + gate1 * hybrid_mixer(h, w_in[l], lower_bounds[l], hgrn_norm_g[l], gmlp_ln_g[l], gmlp_ln_b[l],
                                     spatial_w[l], spatial_b[l], gmlp_norm_g[l], w_out[l])
        h = rmsnorm(x, norm2_g[l]) * (1 + scale2) + shift2
        x = x + gate2 * peer_ffn(h, peer_wq[l], peer_keys[l], peer_u[l], peer_v[l])
    return rmsnorm(x, final_g)
```

```python
import contextlib
import numpy as np
import concourse.bass as bass
import concourse.mybir as mybir
from concourse.bass_utils import run_bass_kernel_spmd

F32 = mybir.dt.float32
BF16 = mybir.dt.bfloat16
U32 = mybir.dt.uint32
AF = mybir.ActivationFunctionType
ALU = mybir.AluOpType
AX = mybir.AxisListType

SEM_ROT = 30000
EPS = 1e-6
D = 1024
NEXP_CH = 128
TB = 256


class Buf:
    __slots__ = ("ap", "key")

    def __init__(self, ap, key):
        self.ap = ap
        self.key = key

    def __getitem__(self, k):
        return self.ap[k]


class Prog:
    ENG = ("tensor", "vector", "scalar", "gpsimd", "sync")

    def __init__(self, nc, es):
        self.nc = nc
        self.es = es
        self.h = {"tensor": nc.tensor, "vector": nc.vector, "scalar": nc.scalar,
                  "gpsimd": nc.gpsimd, "sync": nc.sync}
        self.cnt = {e: 0 for e in self.ENG}
        self.esem = {}
        self.dsem = {}
        self.gv = {}
        self.waited = {e: {} for e in self.ENG}
        self.res = {}
        self.n_wait = 0
        self.n_ins = 0

    def _esem(self, eng, gen):
        k = (eng, gen)
        if k not in self.esem:
            self.esem[k] = self.es.enter_context(self.nc.semaphore("p_%s_%d" % (eng, gen)))
        return self.esem[k]

    def _dsem(self, name):
        if name not in self.dsem:
            self.dsem[name] = [self.es.enter_context(self.nc.semaphore("d_" + name)), 0]
        return self.dsem[name]

    def _keys(self, lst):
        out = []
        for b in lst:
            k = b.key if isinstance(b, Buf) else b
            if isinstance(k, list):
                out.extend(k)
            else:
                out.append(k)
        return out

    def _wait(self, eng, ev, raw=True):
        if ev[0] == "e":
            _, peng, gen, val, pos = ev
            if peng == eng:
                if eng == "tensor" or eng == "sync" or not raw:
                    return
                if self.cnt[eng] - pos >= 2:
                    return
            sk = ("e", peng, gen)
            sem = self._esem(peng, gen)
        else:
            _, name, val = ev
            sk = ("d", name)
            sem = self.dsem[name][0]
        if self.waited[eng].get(sk, 0) >= val:
            return
        self.waited[eng][sk] = val
        self.h[eng].wait_ge(sem, val)
        self.n_wait += 1

    def pre(self, eng, r, w):
        for k in self._keys(r):
            st = self.res.get(k)
            if st and st[0] is not None:
                self._wait(eng, st[0], True)
        for k in self._keys(w):
            st = self.res.get(k)
            if st:
                if st[0] is not None:
                    self._wait(eng, st[0], False)
                for ev in st[1].values():
                    self._wait(eng, ev, False)

    def post(self, eng, ins, r, w, dma=None):
        self.n_ins += 1
        if dma is None:
            self.cnt[eng] += 1
            c = self.cnt[eng]
            gen = c // SEM_ROT
            ins.then_inc(self._esem(eng, gen), 1)
            self.gv[(eng, gen)] = self.gv.get((eng, gen), 0) + 1
            ev = ("e", eng, gen, self.gv[(eng, gen)], c)
            rk = eng
        else:
            ds = self._dsem(dma)
            ds[1] += 16
            ins.then_inc(ds[0], 16)
            ev = ("d", dma, ds[1])
            rk = ("dma", dma, ds[1])
        for k in self._keys(r):
            st = self.res.setdefault(k, [None, {}])
            st[1][rk] = ev
        for k in self._keys(w):
            self.res[k] = [ev, {}]
        return ev

    def op(self, eng, name, r, w, *args, **kw):
        self.pre(eng, r, w)
        ins = getattr(self.h[eng], name)(*args, **kw)
        self.post(eng, ins, r, w)
        return ins

    def dma(self, slot, r, w, out, in_, eng="sync", **kw):
        self._dsem(slot)
        self.pre(eng, r, w)
        ins = self.h[eng].dma_start(out=out, in_=in_, **kw)
        self.post(eng, ins, r, w, dma=slot)
        return ins

    def wait_all_dma(self, eng, slots):
        for s in slots:
            if s in self.dsem and self.dsem[s][1] > 0:
                self._wait(eng, ("d", s, self.dsem[s][1]))


def build_program(NTOK, NPRE, dbg=False):
    nc = bass.Bass("TRN2", target_bir_lowering=False)
    es = contextlib.ExitStack()
    P = Prog(nc, es)
    NT = NTOK // 128
    NB = NTOK // TB
    NPT = NPRE // 128

    def din(name, shape, dt=F32):
        return nc.dram_tensor(name, shape, dt, kind="ExternalInput").ap()

    def sb(name, shape, dt=F32, key=None):
        return Buf(es.enter_context(nc.sbuf_tensor(name, shape, dt)), key or name)

    def V(name, r, w, **kw):
        return P.op("vector", name, r, w, **kw)

    def S(name, r, w, **kw):
        return P.op("scalar", name, r, w, **kw)

    def GP(name, r, w, *a, **kw):
        return P.op("gpsimd", name, r, w, *a, **kw)

    def MM(r, w, out, lhsT, rhs, start=True, stop=True):
        return P.op("tensor", "matmul", r, w, out, lhsT=lhsT, rhs=rhs, start=start, stop=stop)

    def TR(r, w, out, in_, ident):
        return P.op("tensor", "transpose", r, w, out=out, in_=in_, identity=ident)

    def ACT(r, w, out, in_, func, **kw):
        return P.op("scalar", "activation", r, w, out=out, in_=in_, func=func, **kw)

    xs = din("xs", [NTOK, D])
    xp = din("xp", [max(NPRE, 128), D])
    flag = din("flag", [128, 1])
    cT = din("cT", [128, 8])
    ada_w = din("ada_w", [D, 6 * D])
    ada_bT = din("ada_bT", [128, 48])
    abg = din("abg", [128, 2, D])
    n1gT = din("n1gT", [128, 8])
    n2gT = din("n2gT", [128, 8])
    w_in = din("w_in", [D, 3072])
    w_out = din("w_out", [D, D])
    wq = din("wq", [D, 2048])
    lbg = din("lbg", [128, 2, 4])
    vec512 = din("vec512", [128, 4, 512])
    fg_d = din("fg_bc", [128, D])
    spw = din("spw", [4, 128, 128])
    spbT = din("spbT", [128, 4])
    keys = din("keys", [16, 128, 128])
    pu = din("pu", [128 * NEXP_CH, D])
    pv = din("pv", [128 * NEXP_CH, D])
    out_d = nc.dram_tensor("out", [NTOK, D], F32, kind="ExternalOutput").ap()
    win_s = nc.dram_tensor("win_s", [128, 8, 3072], BF16, kind="Internal").ap()
    wout_s = nc.dram_tensor("wout_s", [128, 8, 1024], BF16, kind="Internal").ap()
    wq_s = nc.dram_tensor("wq_s", [128, 8, 2048], BF16, kind="Internal").ap()
    uv_s = nc.dram_tensor("uv_s", [NEXP_CH, 128, 2048], BF16, kind="Internal").ap()
    dbg_d = None
    dbg2_d = None
    if dbg:
        dbg_d = nc.dram_tensor("dbg", [NTOK, D], F32, kind="ExternalOutput").ap()
        dbg2_d = nc.dram_tensor("dbg2", [NTOK, D], BF16, kind="ExternalOutput").ap()

    BK = [Buf(es.enter_context(nc.psum_tensor("bk%d" % i, [128, 512], F32)), "bk%d" % i)
          for i in range(8)]

    def bkb(i):
        return BK[i].ap[:].bitcast(BF16)

    OVK = ["ov%d" % r for r in range(32)]
    OV = sb("ov", [128, 32768], BF16, key=OVK)
    H = [OVK[0:16], OVK[16:32]]
    def wk(kc):
        return OVK[3 * kc:3 * kc + 3]
    ov_f32 = OV.ap[:].bitcast(F32)
    w_in_v = OV.ap[:, 0:24576].rearrange("p (c n) -> p c n", n=3072)
    w_out_v = OV.ap[:, 24576:32768].rearrange("p (c n) -> p c n", n=1024)
    G_v = OV.ap[:].rearrange("p (i t) -> p i t", t=TB)

    ident = sb("ident", [128, 128])
    identb = sb("identb", [128, 128], BF16)
    iotab = sb("iotab", [128, 128], BF16)
    iota16 = sb("iota16", [128, 16])
    maskT = sb("maskT", [128, 128])
    scanm = sb("scanm", [128, 512])
    cact = sb("cact", [128, 8])
    modT = sb("modT", [128, 4, 8])
    QG = sb("qg", [128, 2 * D])
    gate_bc = Buf(QG.ap[:].rearrange("p (a n) -> p a n", n=D), "qg")
    A1 = sb("A1", [128, 8]); A2 = sb("A2", [128, 8])
    smallin = sb("smallin", [128, 80])
    abT = sb("abT", [128, 48])
    lbt = sb("lbt", [128, 4]); l1m = sb("l1m", [128, 4]); lbtmp = sb("lbtmp", [128, 4])
    vecs = sb("vecs", [128, 4, 512])
    fgb = sb("fgb", [128, D])
    keysT = sb("keysT", [128, 16, 128], BF16)
    WT = sb("WT", [128, 4, 128], BF16)
    S32 = sb("S32", [128, 4, 128])
    SbA = sb("SbA", [128, 4, 128], BF16)
    SbB = sb("SbB", [128, 4, 128], BF16)

    xt = [sb("xt%d" % i, [128, D]) for i in range(2)]
    x1 = [sb("x1_%d" % i, [128, D]) for i in range(2)]
    xsb = sb("xsb", [128, D], BF16)
    st4 = sb("st4", [128, 16])
    hT = sb("hT", [128, 8, 128], BF16)

    h2T = sb("h2T", [128, 8, TB], BF16)
    T5 = sb("t5", [128, 6, 512])
    t512 = [Buf(T5.ap[:, i, :], "t5_%d" % i) for i in range(6)]
    kTt = sb("kTt", [128, 4, 128], BF16)
    qTt = sb("qTt", [128, 4, 128], BF16)
    qA = sb("qA", [128, 4, 128], BF16)
    qB = sb("qB", [128, 4, 128], BF16)
    ebl = sb("ebl", [128, 4, 2])
    vb = sb("vb", [128, 512], BF16)
    scm = sb("scm", [128, 4, 128], BF16)
    ktok = sb("ktok", [128, 4, 128], BF16)
    cat = sb("cat", [128, D], BF16)
    catT = sb("catT", [128, 8, 128], BF16)
    hT2 = Buf(catT.ap, "catT")
    vlnb = sb("vlnb", [128, 512], BF16)
    qTs = Buf(QG.ap[:].bitcast(BF16).rearrange("p (g t) -> p g t", t=TB), "qg")
    TK = sb("tk", [128, 4096])
    sim = Buf(TK.ap[:, 0:2048].rearrange("p (g n) -> p g n", n=128), "tk0")
    simw = Buf(TK.ap[:, 2048:4096].rearrange("p (g n) -> p g n", n=128), "tk1")
    cand = Buf(TK.ap[:, 0:2048].rearrange("p (h c) -> p h c", c=256), "tk0")
    candw = Buf(TK.ap[:, 2048:4096].rearrange("p (h c) -> p h c", c=256), "tk1")
    crep = Buf(TK.ap[:, 0:1024].rearrange("p (c m) -> p c m", m=128), "tk0")
    s16 = sb("s16", [128, 16, 16])
    i16 = sb("i16", [128, 16, 16], U32)
    i16f = sb("i16f", [128, 16, 16])
    ts16 = sb("ts16", [128, 8, 16])
    pos = sb("pos", [128, 8, 16], U32)
    pa_u = sb("pa_u", [128, 8, 16], U32); pb_u = sb("pb_u", [128, 8, 16], U32)
    pa_f = sb("pa_f", [128, 8, 16]); pb_f = sb("pb_f", [128, 8, 16])
    gexp = sb("gexp", [128, 8, 16]); gate = sb("gate", [128, 8, 16])
    isel = sb("isel", [128, 8, 16]); jsel = sb("jsel", [128, 8, 16])
    kI = sb("kI", [128, TB], BF16); kJ = sb("kJ", [128, TB], BF16); kG = sb("kG", [128, TB], BF16)
    Pm = [sb("Pm%d" % i, [128, 128], BF16) for i in range(4)]
    Qm = [sb("Qm%d" % i, [128, 128], BF16) for i in range(4)]
    NU = 6
    NV = 6
    UV = sb("uv", [128, NU + NV, 1024], BF16)
    UTs = [Buf(UV.ap[:, i, :], "UTs%d" % i) for i in range(NU)]
    VBs = [Buf(UV.ap[:, NU + i, :], "VBs%d" % i) for i in range(NV)]
    wqs = [Buf(UV.ap[:, 0:2, :].rearrange("p a (c n) -> p (a c) n", n=256), ["UTs0", "UTs1"]),
           Buf(UV.ap[:, NU:NU + 2, :].rearrange("p a (c n) -> p (a c) n", n=256), ["VBs0", "VBs1"])]
    ga = [sb("ga%d" % i, [128, TB]) for i in range(3)]
    Wt = [sb("Wt%d" % i, [128, TB], BF16) for i in range(3)]
    stg = [Buf(T5.ap[:, 2:4, :].rearrange("p a n -> p (a n)"), ["t5_2", "t5_3"]),
           Buf(T5.ap[:, 4:6, :].rearrange("p a n -> p (a n)"), ["t5_4", "t5_5"])]
    print("sbuf bytes/partition left:", nc.sbuf_bytes_remaining)

    def rstd_from_ss(ss_ap, n, out_ap, r, w):
        ACT(r, w, out_ap, ss_ap, AF.Ln, scale=1.0 / n, bias=EPS)
        ACT(w, w, out_ap, out_ap, AF.Exp, scale=-0.5)

    GP("memset", [], [ident], ident[:], 1.0)
    GP("affine_select", [ident], [ident], out=ident[:], in_=ident[:], pattern=[[-1, 128]],
       compare_op=ALU.is_equal, fill=0.0, base=0, channel_multiplier=1)
    V("tensor_copy", [ident], [identb], out=identb[:], in_=ident[:])
    GP("iota", [], [maskT], maskT[:], pattern=[[1, 128]], base=0, channel_multiplier=0,
       allow_small_or_imprecise_dtypes=True)
    V("tensor_copy", [maskT], [iotab], out=iotab[:], in_=maskT[:])
    V("tensor_copy", [maskT], [iota16], out=iota16[:], in_=maskT[:, 0:16])
    GP("memset", [iotab, iota16], [maskT], maskT[:], 1.0)
    GP("affine_select", [maskT], [maskT], out=maskT[:], in_=maskT[:], pattern=[[1, 128]],
       compare_op=ALU.is_ge, fill=0.0, base=0, channel_multiplier=-1)
    GP("memset", [maskT], [maskT], maskT[0:64, 64:128], 0.0)
    GP("memset", [], [scanm], scanm[:], 1.0)
    for c in range(8):
        GP("memset", [scanm], [scanm], scanm[:, c * 64:c * 64 + 1], 0.0)
    GP("memset", [], [S32], S32[:], 0.0)
    GP("memset", [], [SbA], SbA[:], 0.0)
    GP("memset", [], [qA], qA[:], 0.0)
    GP("memset", [], [qB], qB[:], 0.0)

    P.dma("sm", [], [smallin], smallin[:, 0:8], cT[:, :])
    P.dma("sm", [], [smallin], smallin[:, 8:16], n1gT[:, :])
    P.dma("sm", [], [smallin], smallin[:, 16:24], n2gT[:, :])
    P.dma("sm", [], [smallin], smallin[:, 24:32], lbg[:, :, :].rearrange("p a b -> p (a b)"))
    P.dma("sm", [], [smallin], smallin[:, 32:36], spbT[:, :])
    P.dma("sm", [], [smallin], smallin[:, 36:37], flag[:, :])
    P.dma("abT", [], [abT], abT[:], ada_bT[:, :])
    P.dma("gatebc", [], [gate_bc], gate_bc[:], abg[:, :, :])
    P.dma("vecs", [], [vecs], vecs[:], vec512[:, :, :])
    P.dma("fgb", [], [fgb], fgb[:], fg_d[:, :])
    cTs = smallin.ap[:, 0:8]; n1g = smallin.ap[:, 8:16]; n2g = smallin.ap[:, 16:24]
    lbg_s = smallin.ap[:, 24:32]; spb = smallin.ap[:, 32:36]; flg = smallin.ap[:, 36:37]
    hg_bc = vecs.ap[:, 0, :]; lng_bc = vecs.ap[:, 1, :]; lnb_bc = vecs.ap[:, 2, :]; gng_bc = vecs.ap[:, 3, :]

    V("tensor_tensor", [smallin], [lbtmp], out=lbtmp[:], in0=lbg_s[:, 4:8], in1=lbg_s[:, 0:4], op=ALU.subtract)
    ACT([lbtmp], [lbt], lbt[:], lbtmp[:], AF.Exp)
    ACT([lbt], [l1m], l1m[:], lbt[:], AF.Ln, bias=1.0)
    V("tensor_tensor", [lbtmp, l1m], [l1m], out=l1m[:], in0=lbtmp[:], in1=l1m[:], op=ALU.subtract)
    V("tensor_scalar", [lbt], [lbt], out=lbt[:], in0=lbt[:], scalar1=1.0, scalar2=None, op0=ALU.add)
    V("reciprocal", [lbt], [lbt], out=lbt[:], in_=lbt[:])

    ACT([smallin], [cact], cact[:], cTs, AF.Exp, scale=-1.0)
    ACT([cact], [cact], cact[:], cact[:], AF.Ln, bias=1.0)
    ACT([cact], [cact], cact[:], cact[:], AF.Exp, scale=-1.0)
    V("tensor_tensor", [cact, smallin], [cact], out=cact[:], in0=cact[:], in1=cTs, op=ALU.mult)
    for kc in range(8):
        V("tensor_copy", [cact], [crep], out=crep[:, kc, :], in_=cact[:, kc:kc + 1].to_broadcast([128, 128]))

    ada_v = ada_w.rearrange("(c p) n -> p c n", p=128)
    for s in range(6):
        half = s % 2
        stage = ov_f32[:, half * 8192:(half + 1) * 8192].rearrange("p (c n) -> p c n", n=1024)
        key = H[half]
        for kc in range(8):
            P.dma("ovh%d" % half, [], [key], stage[:, kc, :], ada_v[:, kc, s * 1024:(s + 1) * 1024])
        if s in (2, 5):
            gi = 0 if s == 2 else 1
            for hf in range(2):
                for kc in range(8):
                    MM([crep, key], [BK[hf]], BK[hf][:, :], lhsT=crep[:, kc, :],
                       rhs=stage[:, kc, hf * 512:(hf + 1) * 512], start=(kc == 0), stop=(kc == 7))
                V("tensor_tensor", [BK[hf], gate_bc], [gate_bc], out=gate_bc[:, gi, hf * 512:(hf + 1) * 512],
                  in0=BK[hf][:, :], in1=gate_bc[:, gi, hf * 512:(hf + 1) * 512], op=ALU.add)
        else:
            mi = {0: 0, 1: 1, 3: 2, 4: 3}[s]
            for cc in range(8):
                for kc in range(8):
                    MM([cact, key], [BK[2]], BK[2][:, cc:cc + 1], lhsT=stage[:, kc, cc * 128:(cc + 1) * 128],
                       rhs=cact[:, kc:kc + 1], start=(kc == 0), stop=(kc == 7))
            V("tensor_tensor", [BK[2], abT], [modT], out=modT[:, mi, :], in0=BK[2][:, 0:8],
              in1=abT[:, s * 8:(s + 1) * 8], op=ALU.add)
    V("scalar_tensor_tensor", [modT, smallin], [A1], out=A1[:], in0=modT[:, 1, :], scalar=1.0, in1=n1g,
      op0=ALU.add, op1=ALU.mult)
    V("scalar_tensor_tensor", [modT, smallin], [A2], out=A2[:], in0=modT[:, 3, :], scalar=1.0, in1=n2g,
      op0=ALU.add, op1=ALU.mult)
    B1 = modT.ap[:, 0, :]
    B2 = modT.ap[:, 2, :]

    kst = ov_f32[:, 0:2048].rearrange("p (g d) -> p g d", d=128)
    P.dma("ovh0", [], [H[0]], kst, keys.rearrange("g n d -> n g d"))
    for g in range(16):
        b = 3 + (g % 2)
        TR([H[0], ident], [BK[b]], BK[b][:, 0:128], kst[:, g, :], ident[:])
        V("tensor_copy", [BK[b]], [keysT], out=keysT[:, g, :], in_=BK[b][:, 0:128])
    wst = ov_f32[:, 8192:8192 + 512].rearrange("p (g d) -> p g d", d=128)
    P.dma("ovh1", [], [H[1]], wst, spw.rearrange("g t s -> t g s"))
    for g in range(4):
        GP("affine_select", [H[1]], [H[1]], out=wst[:, g, :], in_=wst[:, g, :], pattern=[[-1, 128]],
           compare_op=ALU.is_ge, fill=0.0, base=0, channel_multiplier=1)
        b = 3 + (g % 2)
        TR([H[1], ident], [BK[b]], BK[b][:, 0:128], wst[:, g, :], ident[:])
        V("tensor_copy", [BK[b]], [WT], out=WT[:, g, :], in_=BK[b][:, 0:128])

    def prep_weight(src, ncols, dst, dkey, scale_bc=None):
        sv = src.rearrange("(c p) n -> p c n", p=128)
        for j in range(ncols // 512):
            half = j % 2
            key = H[half]
            slot = "ovh%d" % half
            stage = ov_f32[:, half * 8192:half * 8192 + 4096].rearrange("p (c n) -> p c n", n=512)
            stb = OV.ap[:, half * 16384 + 8192: half * 16384 + 8192 + 4096].rearrange("p (c n) -> p c n", n=512)
            P.dma(slot, [], [key], stage, sv[:, :, j * 512:(j + 1) * 512])
            if scale_bc is None:
                if j % 2 == 0:
                    V("tensor_copy", [key], [key], out=stb, in_=stage)
                else:
                    GP("tensor_copy", [key], [key], out=stb, in_=stage)
            else:
                V("tensor_tensor", [key, gate_bc], [key], out=stb, in0=stage,
                  in1=scale_bc[:, j * 512:(j + 1) * 512].unsqueeze(1).to_broadcast([128, 8, 512]), op=ALU.mult)
            P.dma(slot, [key], [(dkey, j)], dst[:, :, j * 512:(j + 1) * 512], stb)

    prep_weight(w_in, 3072, win_s, "win_s")
    prep_weight(w_out, 1024, wout_s, "wout_s", scale_bc=gate_bc.ap[:, 0, :])
    prep_weight(wq, 2048, wq_s, "wq_s")

    pu_v = pu.rearrange("(i j) d -> i j d", j=128)
    pv_v = pv.rearrange("(i j) d -> i j d", j=128)
    Ust = [Buf(ov_f32[:, 12288 + sl * 1024:12288 + (sl + 1) * 1024], [OVK[24 + 2 * sl], OVK[25 + 2 * sl]]) for sl in range(2)]
    Vst = [Buf(ov_f32[:, 14336 + sl * 1024:14336 + (sl + 1) * 1024], [OVK[28 + 2 * sl], OVK[29 + 2 * sl]]) for sl in range(2)]

    def prep_load(i):
        sl = i % 2
        P.dma("ust%d" % sl, [], [Ust[sl]], Ust[sl][:], pu_v[i])
        P.dma("vst%d" % sl, [], [Vst[sl]], Vst[sl][:], pv_v[i])

    def prep_chunk(i):
        sl = i % 2
        if i + 1 < NEXP_CH:
            prep_load(i + 1)
        us = UTs[i % NU]
        for hb in range(2):
            b = 6 + hb
            for q in range(4):
                dc = hb * 4 + q
                TR([Ust[sl], ident], [BK[b]], BK[b][:, q * 128:(q + 1) * 128], Ust[sl][:, dc * 128:(dc + 1) * 128], ident[:])
            if hb == 0:
                V("tensor_copy", [BK[b]], [us], out=us[:, 0:512], in_=BK[b][:, :])
            else:
                S("copy", [BK[b]], [us], out=us[:, 512:1024], in_=BK[b][:, :])
        P.dma("UTs%d" % (i % NU), [us], [("ut", i)], uv_s[i][:, 0:1024], us[:])
        vs = VBs[i % NV]
        GP("tensor_tensor", [Vst[sl], gate_bc], [vs], out=vs[:], in0=Vst[sl][:], in1=gate_bc[:, 1, :], op=ALU.mult)
        P.dma("VBs%d" % (i % NV), [vs], [("vb", i)], uv_s[i][:, 1024:2048], vs[:])

    def load_win_piece(pc):
        if pc < 8:
            P.dma("ovr%d" % pc, [("win_s", j) for j in range(6)], [wk(pc)], w_in_v[:, pc, :], win_s[:, pc, :])
        else:
            cc = pc - 8
            P.dma("ovr%d" % pc, [("wout_s", j) for j in range(2)], [OVK[24 + cc]], w_out_v[:, cc, :], wout_s[:, cc, :])

    def load_win():
        for pc in range(16):
            load_win_piece(pc)

    h2T_t = [Buf(h2T.ap, "h2T0"), Buf(h2T.ap, "h2T1")]
    st5 = sb("st5", [128, 16])

    def run(gen):
        for _ in gen:
            pass

    def sched(streams):
        done = set()
        pending = list(streams)
        active = []
        while pending or active:
            for st in list(pending):
                if all(d in done for d in st[2]):
                    pending.remove(st)
                    active.append((st[0], st[1]()))
            for ent in list(active):
                try:
                    next(ent[1])
                except StopIteration:
                    active.remove(ent)
                    done.add(ent[0])

    def interleave(ga_, gb_, wa=1, wb=1):
        gens = [[ga_, wa], [gb_, wb]]
        while gens:
            for ent in list(gens):
                for _ in range(ent[1]):
                    try:
                        next(ent[0])
                    except StopIteration:
                        gens.remove(ent)
                        break

    def norm_transpose(src, A, Bv, dstT, col0, bank):
        ACT([src], [xsb, st4], xsb[:], src[:], AF.Square, accum_out=st4[:, 0:1])
        rstd_from_ss(st4[:, 0:1], D, st4[:, 1:2], [st4], [st4])
        V("tensor_scalar", [src, st4], [xsb], out=xsb[:], in0=src[:], scalar1=st4[:, 1:2], scalar2=None, op0=ALU.mult)
        yield
        pt = bkb(bank)
        for dc in range(8):
            TR([xsb, identb], [BK[bank]], pt[:, dc * 128:(dc + 1) * 128], xsb[:, dc * 128:(dc + 1) * 128], identb[:])
        yield
        for dc in range(8):
            ACT([BK[bank], A, modT], [dstT], dstT[:, dc, col0:col0 + 128], pt[:, dc * 128:(dc + 1) * 128],
                AF.Identity, scale=A[:, dc:dc + 1], bias=Bv[:, dc:dc + 1])
        yield

    def hgrn_state_and_scores(full, hT=hT):
        E, sp, l1, zsp, bb = t512[0:5]
        arg = zsp; Eq = E; spq = sp
        pZ = BK[2]; pQ = BK[1]; pV = BK[3]
        for h in range(4):
            for kc in range(8):
                MM([wk(kc), hT], [pZ], pZ[:, h * 128:(h + 1) * 128], lhsT=w_in_v[:, kc, 512 + h * 128:512 + (h + 1) * 128],
                   rhs=hT[:, kc, :], start=(kc == 0), stop=(kc == 7))
            if h % 2 == 1:
                yield
        for kc in range(8):
            MM([wk(kc), hT], [pV], pV[:, :], lhsT=hT[:, kc, :], rhs=w_in_v[:, kc, 1024:1536], start=(kc == 0), stop=(kc == 7))
        yield
        if full:
            for h in range(4):
                for kc in range(8):
                    MM([wk(kc), hT], [pQ], pQ[:, h * 128:(h + 1) * 128], lhsT=w_in_v[:, kc, h * 128:(h + 1) * 128],
                       rhs=hT[:, kc, :], start=(kc == 0), stop=(kc == 7))
                if h % 2 == 1:
                    yield
        ACT([pZ], [E], E[:], pZ[:, :], AF.Exp, scale=-1.0)
        ACT([E], [sp], sp[:], E[:], AF.Ln, bias=1.0)
        for h in range(4):
            ACT([E, lbt], [l1], l1[:, h * 128:(h + 1) * 128], E[:, h * 128:(h + 1) * 128], AF.Ln,
                scale=lbt[:, h:h + 1], bias=1.0)
        S("copy", [pV], [vb], out=vb[:], in_=pV[:, :])
        yield
        V("tensor_tensor", [pZ, sp], [zsp], out=zsp[:], in0=pZ[:, :], in1=sp[:], op=ALU.add)
        V("tensor_tensor", [l1, sp], [l1], out=l1[:], in0=l1[:], in1=sp[:], op=ALU.subtract)
        V("tensor_tensor_scan", [scanm, l1], [bb], out=bb[:], data0=scanm[:], data1=l1[:], initial=0.0,
          op0=ALU.mult, op1=ALU.add)
        V("tensor_tensor", [zsp, bb], [arg], out=arg[:], in0=zsp[:], in1=bb[:], op=ALU.add)
        yield
        for h in range(4):
            ACT([arg, l1m], [kTt], kTt[:, h, :], arg[:, h * 128:(h + 1) * 128], AF.Exp, scale=-1.0,
                bias=l1m[:, h:h + 1])
        bb3 = bb.ap[:].rearrange("p (h t) -> p h t", t=128)
        ACT([bb], [ebl], ebl[:], bb3[:, :, 63:128:64], AF.Exp)
        yield
        if full:
            ACT([pQ], [Eq], Eq[:], pQ[:, :], AF.Exp, scale=-1.0)
            ACT([Eq], [spq], spq[:], Eq[:], AF.Ln, bias=1.0)
            V("tensor_tensor", [bb, spq], [spq], out=spq[:], in0=bb[:], in1=spq[:], op=ALU.subtract)
            ACT([spq], [Eq], Eq[:], spq[:], AF.Exp)
            V("tensor_tensor", [pQ, Eq], [qTt], out=qTt[:].rearrange("p h t -> p (h t)"), in0=pQ[:, :], in1=Eq[:], op=ALU.mult)
            GP("tensor_copy", [qTt], [qA], out=qA[:, :, 0:64], in_=qTt[:, :, 0:64])
            GP("tensor_copy", [qTt], [qB], out=qB[:, :, 64:128], in_=qTt[:, :, 64:128])
            yield
        pK = bkb(0)
        for h in range(4):
            TR([kTt, identb], [BK[0]], pK[:, h * 128:(h + 1) * 128], kTt[:, h, :], identb[:])
        S("copy", [BK[0]], [ktok], out=ktok[:].rearrange("p h k -> p (h k)"), in_=pK[:, 0:512])
        yield
        pO = BK[2]; pS = BK[1]
        if full:
            for h in range(4):
                MM([kTt, qTt], [pS], pS[:, h * 128:(h + 1) * 128], lhsT=kTt[:, h, :], rhs=qTt[:, h, :])
            V("tensor_tensor", [pS, maskT], [scm], out=scm[:], in0=pS[:, :].rearrange("p (h t) -> p h t", t=128),
              in1=maskT[:].unsqueeze(1).to_broadcast([128, 4, 128]), op=ALU.mult)
            yield
        pSt = BK[5]
        for c in range(2):
            lo = c * 64
            for h in range(4):
                MM([ktok, vb], [pSt], pSt[:, h * 128:(h + 1) * 128], lhsT=ktok[lo:lo + 64, h, :],
                   rhs=vb[lo:lo + 64, h * 128:(h + 1) * 128])
            V("tensor_tensor", [S32, pSt], [S32], out=S32[:].rearrange("p h v -> p (h v)"),
              in0=S32[:].rearrange("p h v -> p (h v)"), in1=pSt[:, :], op=ALU.add)
            V("tensor_tensor", [S32, ebl], [S32], out=S32[:], in0=S32[:],
              in1=ebl[:, :, c:c + 1].to_broadcast([128, 4, 128]), op=ALU.mult)
            dst = SbB if c == 0 else SbA
            if c == 0 and full:
                S("copy", [S32], [dst], out=dst[:], in_=S32[:])
                yield
                for h in range(4):
                    MM([scm, vb], [pO], pO[:, h * 128:(h + 1) * 128], lhsT=scm[:, h, :], rhs=vb[:, h * 128:(h + 1) * 128],
                       start=True, stop=False)
                    MM([qA, SbA], [pO], pO[:, h * 128:(h + 1) * 128], lhsT=qA[:, h, :], rhs=SbA[:, h, :],
                       start=False, stop=False)
                    MM([qB, SbB], [pO], pO[:, h * 128:(h + 1) * 128], lhsT=qB[:, h, :], rhs=SbB[:, h, :],
                       start=False, stop=True)
            elif full:
                GP("tensor_copy", [S32], [dst], out=dst[:], in_=S32[:])
            yield

    st6 = sb("st6", [128, 16])

    def par(*gens_):
        gl = list(gens_)
        while gl:
            for g_ in list(gl):
                try:
                    next(g_)
                except StopIteration:
                    gl.remove(g_)
            yield

    def mixer_H():
        yield from hgrn_state_and_scores(True)
        pO = BK[2]
        a0 = t512[0]
        pOG = BK[3]
        for kc in range(8):
            MM([wk(kc), hT], [pOG], pOG[:, :], lhsT=hT[:, kc, :], rhs=w_in_v[:, kc, 1536:2048], start=(kc == 0), stop=(kc == 7))
        yield
        for h in range(4):
            ACT([pO], [scm, st4], scm[:, h, :], pO[:, h * 128:(h + 1) * 128], AF.Square, accum_out=st4[:, 4 + h:5 + h])
        rstd_from_ss(st4[:, 4:8], 128, st4[:, 8:12], [st4], [st4])
        yield
        ACT([pOG], [a0], a0[:], pOG[:, :], AF.Exp, scale=-1.0)
        ACT([a0], [a0], a0[:], a0[:], AF.Ln, bias=1.0)
        ACT([a0], [a0], a0[:], a0[:], AF.Exp, scale=-1.0)
        V("tensor_tensor", [pOG, a0], [a0], out=a0[:], in0=pOG[:, :], in1=a0[:], op=ALU.mult)
        V("tensor_tensor", [a0, vecs], [a0], out=a0[:], in0=a0[:], in1=hg_bc, op=ALU.mult)
        yield
        for h in range(4):
            V("scalar_tensor_tensor", [pO, st4, a0], [cat], out=cat[:, h * 128:(h + 1) * 128],
              in0=pO[:, h * 128:(h + 1) * 128], scalar=st4[:, 8 + h:9 + h], in1=a0[:, h * 128:(h + 1) * 128],
              op0=ALU.mult, op1=ALU.mult)
        yield

    def mixer_M(x1o):
        a1 = Buf(x1o.ap[:, 0:512], x1o.key)
        a2 = Buf(x1o.ap[:, 512:1024], x1o.key)
        a3 = t512[5]
        pU = BK[4]; pVG = BK[4]; pM = BK[4]
        for kc in range(8):
            MM([wk(kc), hT], [pU], pU[:, :], lhsT=hT[:, kc, :], rhs=w_in_v[:, kc, 2048:2560], start=(kc == 0), stop=(kc == 7))
        yield
        ACT([pU], [a1], a1[:], pU[:, :], AF.Gelu_apprx_tanh)
        for kc in range(8):
            MM([wk(kc), hT], [pVG], pVG[:, :], lhsT=hT[:, kc, :], rhs=w_in_v[:, kc, 2560:3072], start=(kc == 0), stop=(kc == 7))
        yield
        ACT([pVG], [a2], a2[:], pVG[:, :], AF.Gelu_apprx_tanh)
        V("tensor_reduce", [a2], [st6], out=st6[:, 2:3], in_=a2[:], axis=AX.X, op=ALU.add)
        V("tensor_scalar", [st6], [st6], out=st6[:, 3:4], in0=st6[:, 2:3], scalar1=1.0 / 512, scalar2=None, op0=ALU.mult)
        V("tensor_scalar", [a2, st6], [a2], out=a2[:], in0=a2[:], scalar1=st6[:, 3:4], scalar2=None, op0=ALU.subtract)
        yield
        ACT([a2], [vlnb, st6], vlnb[:], a2[:], AF.Square, accum_out=st6[:, 12:13])
        rstd_from_ss(st6[:, 12:13], 512, st6[:, 13:14], [st6], [st6])
        V("scalar_tensor_tensor", [a2, st6, vecs], [a2], out=a2[:], in0=a2[:], scalar=st6[:, 13:14], in1=lng_bc,
          op0=ALU.mult, op1=ALU.mult)
        V("tensor_tensor", [a2, vecs], [vlnb], out=vlnb[:], in0=a2[:], in1=lnb_bc, op=ALU.add)
        yield
        for g in range(4):
            MM([WT, vlnb], [pM], pM[:, g * 128:(g + 1) * 128], lhsT=WT[:, g, :], rhs=vlnb[:, g * 128:(g + 1) * 128])
        for g in range(4):
            V("scalar_tensor_tensor", [pM, smallin, a1], [a3], out=a3[:, g * 128:(g + 1) * 128],
              in0=pM[:, g * 128:(g + 1) * 128], scalar=spb[:, g:g + 1], in1=a1[:, g * 128:(g + 1) * 128],
              op0=ALU.add, op1=ALU.mult)
        yield
        for g in range(4):
            ACT([a3], [cat, st6], cat[:, 512 + g * 128:512 + (g + 1) * 128], a3[:, g * 128:(g + 1) * 128], AF.Square,
                accum_out=st6[:, 4 + g:5 + g])
        rstd_from_ss(st6[:, 4:8], 128, st6[:, 8:12], [st6], [st6])
        for g in range(4):
            V("scalar_tensor_tensor", [a3, st6, vecs], [cat], out=cat[:, 512 + g * 128:512 + (g + 1) * 128],
              in0=a3[:, g * 128:(g + 1) * 128], scalar=st6[:, 8 + g:9 + g], in1=gng_bc[:, g * 128:(g + 1) * 128],
              op0=ALU.mult, op1=ALU.mult)
        yield

    def mixer_tile(xin, x1o, tt, drow):
        yield from norm_transpose(xin, A1, B1, hT, 0, 0)
        yield from par(mixer_H(), mixer_M(x1o))
        if dbg:
            P.dma("dbg2", [cat], [], dbg2_d[drow:drow + 128, :], cat[:])
        pt = bkb(0)
        for cc in range(8):
            TR([cat, identb], [BK[0]], pt[:, cc * 128:(cc + 1) * 128], cat[:, cc * 128:(cc + 1) * 128], identb[:])
        S("copy", [BK[0]], [catT], out=catT[:].rearrange("p c t -> p (c t)"), in_=pt[:, :])
        yield
        for hf in range(2):
            b = (5, 1)[hf]
            for cc in range(8):
                MM([OVK[24 + cc], catT], [BK[b]], BK[b][:, :], lhsT=catT[:, cc, :], rhs=w_out_v[:, cc, hf * 512:(hf + 1) * 512],
                   start=(cc == 0), stop=(cc == 7))
            V("tensor_tensor", [xin, BK[b]], [x1o], out=x1o[:, hf * 512:(hf + 1) * 512],
              in0=xin[:, hf * 512:(hf + 1) * 512], in1=BK[b][:, :], op=ALU.add)
            yield
        yield from norm_transpose(x1o, A2, B2, h2T_t[tt], tt * 128, 0)

    def peer_q(tt):
        c0 = tt * 128
        for g2 in range(8):
            ws = wqs[g2 % 2]
            P.dma("wqs%d" % (g2 % 2), [("wq_s", j) for j in range(4)], [ws], ws[:], wq_s[:, :, g2 * 256:(g2 + 1) * 256])
            for gg in range(2):
                g = g2 * 2 + gg
                b = 6
                for kc in range(8):
                    MM([ws, h2T_t[tt]], [BK[b]], BK[b][:, 0:128], lhsT=ws[:, kc, gg * 128:(gg + 1) * 128],
                       rhs=h2T[:, kc, c0:c0 + 128], start=(kc == 0), stop=(kc == 7))
                S("copy", [BK[b]], [qTs], out=qTs[:, g, c0:c0 + 128], in_=BK[b][:, 0:128])
            yield

    def peer_topk(tt):
        c0 = tt * 128
        for qd in range(4):
            b = 7
            for j in range(4):
                g = qd * 4 + j
                MM([qTs, keysT], [BK[b]], BK[b][:, j * 128:(j + 1) * 128], lhsT=qTs[:, g, c0:c0 + 128], rhs=keysT[:, g, :])
            S("copy", [BK[b]], [sim], out=sim[:, qd * 4:(qd + 1) * 4, :].rearrange("p g n -> p (g n)"), in_=BK[b][:, :])
            yield
        for g in range(16):
            V("max", [sim], [s16], out=s16[:, g, 0:8], in_=sim[:, g, :])
            if g % 4 == 3:
                yield
        for g in range(16):
            V("max_index", [sim, s16], [i16], out=i16[:, g, 0:8], in_max=s16[:, g, 0:8], in_values=sim[:, g, :])
            if g % 4 == 3:
                yield
        for g in range(16):
            V("match_replace", [sim, s16], [simw], out=simw[:, g, :], in_to_replace=s16[:, g, 0:8],
              in_values=sim[:, g, :], imm_value=-1e30)
            if g % 4 == 3:
                yield
        for g in range(16):
            V("max", [simw], [s16], out=s16[:, g, 8:16], in_=simw[:, g, :])
            if g % 4 == 3:
                yield
        for g in range(16):
            V("max_index", [simw, s16], [i16], out=i16[:, g, 8:16], in_max=s16[:, g, 8:16], in_values=simw[:, g, :])
            if g % 4 == 3:
                yield
        V("tensor_copy", [i16], [i16f], out=i16f[:], in_=i16[:])
        s16v = s16.ap[:].rearrange("p (h q) a -> p h q a", q=2)
        GP("tensor_tensor", [s16], [cand], out=cand[:].rearrange("p h (a b) -> p h a b", b=16),
          in0=s16v[:, :, 0, :].unsqueeze(3).to_broadcast([128, 8, 16, 16]),
          in1=s16v[:, :, 1, :].unsqueeze(2).to_broadcast([128, 8, 16, 16]), op=ALU.add)
        yield
        for h in range(8):
            V("max", [cand], [ts16], out=ts16[:, h, 0:8], in_=cand[:, h, :])
            if h == 3:
                yield
        yield
        for h in range(8):
            V("max_index", [cand, ts16], [pos], out=pos[:, h, 0:8], in_max=ts16[:, h, 0:8], in_values=cand[:, h, :])
        yield
        for h in range(8):
            V("match_replace", [cand, ts16], [candw], out=candw[:, h, :], in_to_replace=ts16[:, h, 0:8],
              in_values=cand[:, h, :], imm_value=-1e30)
        yield
        for h in range(8):
            V("max", [candw], [ts16], out=ts16[:, h, 8:16], in_=candw[:, h, :])
            if h == 3:
                yield
        yield
        for h in range(8):
            V("max_index", [candw, ts16], [pos], out=pos[:, h, 8:16], in_max=ts16[:, h, 8:16], in_values=candw[:, h, :])
        yield
        V("tensor_tensor", [ts16], [gexp], out=gexp[:], in0=ts16[:], in1=ts16[:, :, 0:1].to_broadcast([128, 8, 16]),
          op=ALU.subtract)
        ACT([gexp], [gexp], gexp[:], gexp[:], AF.Exp)
        V("tensor_reduce", [gexp], [st5], out=st5[:, 8:16], in_=gexp[:], axis=AX.X, op=ALU.add)
        V("reciprocal", [st5], [st5], out=st5[:, 8:16], in_=st5[:, 8:16])
        V("tensor_tensor", [gexp, st5], [gate], out=gate[:], in0=gexp[:],
          in1=st5[:, 8:16].unsqueeze(2).to_broadcast([128, 8, 16]), op=ALU.mult)
        yield
        V("tensor_single_scalar", [pos], [pa_u], out=pa_u[:], in_=pos[:], scalar=4, op=ALU.logical_shift_right)
        V("tensor_single_scalar", [pos], [pb_u], out=pb_u[:], in_=pos[:], scalar=15, op=ALU.bitwise_and)
        V("tensor_copy", [pa_u], [pa_f], out=pa_f[:], in_=pa_u[:])
        V("tensor_copy", [pb_u], [pb_f], out=pb_f[:], in_=pb_u[:])
        yield
        i16v = i16f.ap[:].rearrange("p (h q) a -> p h q a", q=2)
        e4i = cand.ap[:].rearrange("p h (r a) -> p h r a", a=16)
        e4j = candw.ap[:].rearrange("p h (r a) -> p h r a", a=16)
        io4 = iota16[:].unsqueeze(1).unsqueeze(1).to_broadcast([128, 8, 16, 16])
        V("tensor_tensor", [iota16, pa_f], [cand], out=e4i, in0=io4,
          in1=pa_f[:].unsqueeze(3).to_broadcast([128, 8, 16, 16]), op=ALU.is_equal)
        V("tensor_tensor", [iota16, pb_f], [candw], out=e4j, in0=io4,
          in1=pb_f[:].unsqueeze(3).to_broadcast([128, 8, 16, 16]), op=ALU.is_equal)
        yield
        GP("tensor_tensor", [candw, i16f], [candw], out=e4j, in0=e4j,
           in1=i16v[:, :, 1, :].unsqueeze(2).to_broadcast([128, 8, 16, 16]), op=ALU.mult)
        V("tensor_tensor", [cand, i16f], [cand], out=e4i, in0=e4i,
          in1=i16v[:, :, 0, :].unsqueeze(2).to_broadcast([128, 8, 16, 16]), op=ALU.mult)
        V("tensor_reduce", [cand], [isel], out=isel[:], in_=e4i, axis=AX.X, op=ALU.add)
        yield
        V("tensor_reduce", [candw], [jsel], out=jsel[:], in_=e4j, axis=AX.X, op=ALU.add)
        yield
        for (src, dstk, b) in ((isel, kI, 7), (jsel, kJ, 7), (gate, kG, 7)):
            TR([src, ident], [BK[b]], BK[b][:, 0:128], src[:].rearrange("p h r -> p (h r)"), ident[:])
            S("copy", [BK[b]], [dstk], out=dstk[:, c0:c0 + 128], in_=BK[b][:, 0:128])
        yield

    def gbuild(t4lo, t4hi):
        for t4 in range(t4lo, t4hi):
            b = t4 % 4
            for q in range(4):
                t = t4 * 4 + q
                sl = t % 4
                V("tensor_scalar", [iotab, kI], [Pm[sl]], out=Pm[sl][:], in0=iotab[:], scalar1=kI[:, t:t + 1],
                  scalar2=None, op0=ALU.is_equal)
                V("tensor_scalar", [iotab, kJ, kG], [Qm[sl]], out=Qm[sl][:], in0=iotab[:], scalar1=kJ[:, t:t + 1],
                  scalar2=kG[:, t:t + 1], op0=ALU.is_equal, op1=ALU.mult)
                MM([Pm[sl], Qm[sl]], [BK[b]], BK[b][:, q * 128:(q + 1) * 128], lhsT=Qm[sl][:], rhs=Pm[sl][:])
            tg = t4 * 4
            S("copy", [BK[b]], [OV], out=G_v[:, :, tg:tg + 4], in_=BK[b][:, :].rearrange("p (t i) -> p i t", i=128))
            yield

    dbg_row = [0]
    for pc in range(8):
        load_win_piece(pc)
    prep_load(0)
    n_prep = 0
    cpt = -(-NEXP_CH // max(NPT, 1))
    hTp = [hT, hT2]

    def prefix_front(ti):
        sl_ = ti % 2
        P.dma("xt%d" % sl_, [], [xt[sl_]], xt[sl_][:], xp[ti * 128:(ti + 1) * 128, :])
        yield from norm_transpose(xt[sl_], A1, B1, hTp[sl_], 0, 4)

    def prefix_back(ti):
        yield from hgrn_state_and_scores(False, hTp[ti % 2])

    def prep_some(n):
        nonlocal_n = [0]
        for _ in range(n):
            if n_prep_box[0] < NEXP_CH:
                prep_chunk(n_prep_box[0])
                n_prep_box[0] += 1
            yield

    n_prep_box = [0]
    if NPT > 0:
        run(prefix_front(0))
    for ti in range(NPT):
        if ti + 1 < NPT:
            interleave(prefix_back(ti), prefix_front(ti + 1))
        else:
            run(prefix_back(ti))
        run(prep_some(cpt))
    n_prep = n_prep_box[0]
    while n_prep < NEXP_CH:
        prep_chunk(n_prep)
        n_prep += 1
    for pc in range(8, 16):
        load_win_piece(pc)
    V("tensor_scalar", [S32, smallin], [S32], out=S32[:], in0=S32[:], scalar1=flg, scalar2=None, op0=ALU.mult)
    GP("tensor_copy", [S32], [SbA], out=SbA[:], in_=S32[:])

    n_out = 0
    for blk in range(NB):
        if blk == 0:
            for tt in range(2):
                P.dma("xt%d" % tt, [], [xt[tt]], xt[tt][:], xs[tt * 128:(tt + 1) * 128, :])
        t0i = blk * 2
        run(mixer_tile(xt[0], x1[0], 0, t0i * 128))
        def chain(*gens):
            for g_ in gens:
                yield from g_

        def xprefetch():
            if blk + 1 < NB:
                for tt in range(2):
                    ti = (blk + 1) * 2 + tt
                    P.dma("xt%d" % tt, [], [xt[tt]], xt[tt][:], xs[ti * 128:(ti + 1) * 128, :])
            if dbg:
                for tt in range(2):
                    ti = blk * 2 + tt
                    P.dma("dbg", [x1[tt]], [], dbg_d[ti * 128:(ti + 1) * 128, :], x1[tt][:])
            return
            yield

        sched([
            ("m1", lambda: chain(mixer_tile(xt[1], x1[1], 1, (t0i + 1) * 128), xprefetch()), []),
            ("q0", lambda: peer_q(0), []),
            ("k0", lambda: peer_topk(0), ["q0"]),
            ("q1", lambda: peer_q(1), ["m1", "q0"]),
            ("k1", lambda: peer_topk(1), ["q1", "k0"]),
            ("g0", lambda: gbuild(0, 32), ["k0", "m1"]),
            ("g1", lambda: gbuild(32, 64), ["k1", "g0"]),
        ])
        def load_ut(i):
            sl = i % NU
            P.dma("UTs%d" % sl, [("ut", i)], [UTs[sl]], UTs[sl][:], uv_s[i][:, 0:1024])

        def load_vb(i):
            sl = i % NV
            P.dma("VBs%d" % sl, [("vb", i)], [VBs[sl]], VBs[sl][:], uv_s[i][:, 1024:2048])

        def issue_A(i):
            sl = i % NU
            b = i % 3
            for dc in range(8):
                MM([UTs[sl], h2T_t[0], h2T_t[1]], [BK[b]], BK[b][:, 0:TB], lhsT=UTs[sl][:, dc * 128:(dc + 1) * 128], rhs=h2T[:, dc, :],
                   start=(dc == 0), stop=(dc == 7))
            ACT([BK[b]], [ga[b]], ga[b][:], BK[b][:, 0:TB], AF.Gelu_apprx_tanh)
            V("tensor_tensor", [ga[b], OVK[i // 4]], [Wt[b]], out=Wt[b][:], in0=ga[b][:], in1=G_v[:, i, :], op=ALU.mult)

        def issue_Y(i):
            sl = i % NV
            b = i % 3
            for ts_ in range(2):
                for hf in range(2):
                    yb = 4 + ts_ * 2 + hf
                    MM([Wt[b], VBs[sl]], [BK[yb]], BK[yb][:, :], lhsT=Wt[b][:, ts_ * 128:(ts_ + 1) * 128],
                       rhs=VBs[sl][:, hf * 512:(hf + 1) * 512], start=(i == 0), stop=(i == NEXP_CH - 1))

        for i0 in range(NU - 1):
            load_ut(i0)
            if i0 < NV - 1:
                load_vb(i0)
        issue_A(0); issue_A(1)
        for i in range(NEXP_CH):
            if i + NU - 1 < NEXP_CH:
                load_ut(i + NU - 1)
            if i + NV - 1 < NEXP_CH:
                load_vb(i + NV - 1)
            if i + 2 < NEXP_CH:
                issue_A(i + 2)
            issue_Y(i)
            if blk + 1 < NB:
                if i < 96 and i % 12 == 11:
                    load_win_piece(i // 12)
                elif i >= 96 and i % 4 == 3:
                    load_win_piece(8 + (i - 96) // 4)
        for tt in range(2):
            ti = blk * 2 + tt
            for hf in range(2):
                yb = 4 + tt * 2 + hf
                V("tensor_tensor", [x1[tt], BK[yb]], [x1[tt]], out=x1[tt][:, hf * 512:(hf + 1) * 512],
                  in0=x1[tt][:, hf * 512:(hf + 1) * 512], in1=BK[yb][:, :], op=ALU.add)
            so = stg[n_out % 2]
            ACT([x1[tt]], [so, st4], so[:], x1[tt][:], AF.Square, accum_out=st4[:, 0:1])
            rstd_from_ss(st4[:, 0:1], D, st4[:, 1:2], [st4], [st4])
            V("scalar_tensor_tensor", [x1[tt], st4, fgb], [so], out=so[:], in0=x1[tt][:], scalar=st4[:, 1:2], in1=fgb[:],
              op0=ALU.mult, op1=ALU.mult)
            P.dma("stg%d" % (n_out % 2), [so], [], out_d[ti * 128:(ti + 1) * 128, :], so[:])
            n_out += 1
    P.wait_all_dma("sync", ["stg0", "stg1", "dbg", "dbg2"])
    es.close()
    print("instructions:", P.n_ins, "waits:", P.n_wait, "per-engine:", P.cnt)
    return nc


def make_in_maps(inputs, n_cores, ntok, npre):
    f = lambda a: np.ascontiguousarray(np.asarray(a, dtype=np.float32))
    x = f(inputs["x"]); c = f(inputs["c"])
    Bn, Sn, _ = x.shape
    halves = Sn // ntok
    fm = lambda v: np.ascontiguousarray(v.reshape(-1, 128).T)
    bc = lambda v: np.ascontiguousarray(np.broadcast_to(v[None, :], (128, v.shape[0])))
    ada_b = f(inputs["ada_b"])[0]
    lbg = f(inputs["lb_gamma"])
    common = {
        "ada_w": f(inputs["ada_w"])[0],
        "ada_bT": fm(ada_b),
        "abg": np.ascontiguousarray(np.stack([bc(ada_b[2048:3072]), bc(ada_b[5120:6144])], axis=1)),
        "n1gT": fm(f(inputs["norm1_g"])[0]),
        "n2gT": fm(f(inputs["norm2_g"])[0]),
        "w_in": f(inputs["w_in"])[0],
        "w_out": f(inputs["w_out"])[0],
        "wq": f(inputs["peer_wq"])[0],
        "lbg": np.ascontiguousarray(lbg.reshape(2, 4, 128).transpose(2, 0, 1)),
        "vec512": np.ascontiguousarray(np.stack([bc(f(inputs["hgrn_norm_g"])[0]), bc(f(inputs["gmlp_ln_g"])[0]),
                                                 bc(f(inputs["gmlp_ln_b"])[0]), bc(f(inputs["gmlp_norm_g"])[0])], axis=1)),
        "fg_bc": bc(f(inputs["final_g"])),
        "spw": f(inputs["spatial_w"])[0],
        "spbT": np.ascontiguousarray(f(inputs["spatial_b"])[0].T),
        "keys": np.ascontiguousarray(f(inputs["peer_keys"])[0].reshape(16, 128, 128)),
        "pu": f(inputs["peer_u"])[0],
        "pv": f(inputs["peer_v"])[0],
    }
    maps = []
    for core in range(n_cores):
        b = core // halves
        hf = core % halves
        m = dict(common)
        m["xs"] = np.ascontiguousarray(x[b, hf * ntok:(hf + 1) * ntok])
        if npre > 0:
            m["xp"] = np.ascontiguousarray(x[b, 0:npre])
        else:
            m["xp"] = np.ascontiguousarray(x[b, 0:128])
        m["flag"] = np.full((128, 1), 1.0 if hf > 0 else 0.0, np.float32)
        m["cT"] = fm(c[b])
        maps.append(m)
    return maps


def kernel(**inputs):
    x = np.asarray(inputs["x"])
    Bn, Sn, Dm = x.shape
    n_cores = 8
    ntok = Bn * Sn // n_cores
    halves = Sn // ntok
    npre = ntok if halves > 1 else 0
    nc = build_program(ntok, npre)
    maps = make_in_maps(inputs, n_cores, ntok, npre)
    res = run_bass_kernel_spmd(nc, maps, core_ids=list(range(n_cores)))
    out = np.empty((Bn, Sn, Dm), np.float32)
    for core in range(n_cores):
        b = core // halves
        hf = core % halves
        out[b, hf * ntok:(hf + 1) * ntok] = res.results[core]["out"]
    return out
```

```python
import contextlib
import numpy as np
import concourse.bass as bass
import concourse.mybir as mybir
from concourse.bass_utils import run_bass_kernel_spmd

F32 = mybir.dt.float32
BF16 = mybir.dt.bfloat16
U32 = mybir.dt.uint32
AF = mybir.ActivationFunctionType
ALU = mybir.AluOpType
AX = mybir.AxisListType

SEM_ROT = 30000
EPS = 1e-6
D = 1024
NEXP_CH = 128
TB = 256


class Buf:
    __slots__ = ("ap", "key")

    def __init__(self, ap, key):
        self.ap = ap
        self.key = key

    def __getitem__(self, k):
        return self.ap[k]


class Prog:
    ENG = ("tensor", "vector", "scalar", "gpsimd", "sync")

    def __init__(self, nc, es):
        self.nc = nc
        self.es = es
        self.h = {"tensor": nc.tensor, "vector": nc.vector, "scalar": nc.scalar,
                  "gpsimd": nc.gpsimd, "sync": nc.sync}
        self.cnt = {e: 0 for e in self.ENG}
        self.esem = {}
        self.dsem = {}
        self.gv = {}
        self.waited = {e: {} for e in self.ENG}
        self.res = {}
        self.n_wait = 0
        self.n_ins = 0

    def _esem(self, eng, gen):
        k = (eng, gen)
        if k not in self.esem:
            self.esem[k] = self.es.enter_context(self.nc.semaphore("p_%s_%d" % (eng, gen)))
        return self.esem[k]

    def _dsem(self, name):
        if name not in self.dsem:
            self.dsem[name] = [self.es.enter_context(self.nc.semaphore("d_" + name)), 0]
        return self.dsem[name]

    def _keys(self, lst):
        out = []
        for b in lst:
            k = b.key if isinstance(b, Buf) else b
            if isinstance(k, list):
                out.extend(k)
            else:
                out.append(k)
        return out

    def _wait(self, eng, ev, raw=True):
        if ev[0] == "e":
            _, peng, gen, val, pos = ev
            if peng == eng:
                if eng == "tensor" or eng == "sync" or not raw:
                    return
                if self.cnt[eng] - pos >= 2:
                    return
            sk = ("e", peng, gen)
            sem = self._esem(peng, gen)
        else:
            _, name, val = ev
            sk = ("d", name)
            sem = self.dsem[name][0]
        if self.waited[eng].get(sk, 0) >= val:
            return
        self.waited[eng][sk] = val
        self.h[eng].wait_ge(sem, val)
        self.n_wait += 1

    def pre(self, eng, r, w):
        for k in self._keys(r):
            st = self.res.get(k)
            if st and st[0] is not None:
                self._wait(eng, st[0], True)
        for k in self._keys(w):
            st = self.res.get(k)
            if st:
                if st[0] is not None:
                    self._wait(eng, st[0], False)
                for ev in st[1].values():
                    self._wait(eng, ev, False)

    def post(self, eng, ins, r, w, dma=None):
        self.n_ins += 1
        if dma is None:
            self.cnt[eng] += 1
            c = self.cnt[eng]
            gen = c // SEM_ROT
            ins.then_inc(self._esem(eng, gen), 1)
            self.gv[(eng, gen)] = self.gv.get((eng, gen), 0) + 1
            ev = ("e", eng, gen, self.gv[(eng, gen)], c)
            rk = eng
        else:
            ds = self._dsem(dma)
            ds[1] += 16
            ins.then_inc(ds[0], 16)
            ev = ("d", dma, ds[1])
            rk = ("dma", dma, ds[1])
        for k in self._keys(r):
            st = self.res.setdefault(k, [None, {}])
            st[1][rk] = ev
        for k in self._keys(w):
            self.res[k] = [ev, {}]
        return ev

    def op(self, eng, name, r, w, *args, **kw):
        self.pre(eng, r, w)
        ins = getattr(self.h[eng], name)(*args, **kw)
        self.post(eng, ins, r, w)
        return ins

    def dma(self, slot, r, w, out, in_, eng="sync", **kw):
        self._dsem(slot)
        self.pre(eng, r, w)
        ins = self.h[eng].dma_start(out=out, in_=in_, **kw)
        self.post(eng, ins, r, w, dma=slot)
        return ins

    def wait_all_dma(self, eng, slots):
        for s in slots:
            if s in self.dsem and self.dsem[s][1] > 0:
                self._wait(eng, ("d", s, self.dsem[s][1]))


def build_program(NTOK, NPRE, dbg=False):
    nc = bass.Bass("TRN2", target_bir_lowering=False)
    es = contextlib.ExitStack()
    P = Prog(nc, es)
    NT = NTOK // 128
    NB = NTOK // TB
    NPT = NPRE // 128

    def din(name, shape, dt=F32):
        return nc.dram_tensor(name, shape, dt, kind="ExternalInput").ap()

    def sb(name, shape, dt=F32, key=None):
        return Buf(es.enter_context(nc.sbuf_tensor(name, shape, dt)), key or name)

    def V(name, r, w, **kw):
        return P.op("vector", name, r, w, **kw)

    def S(name, r, w, **kw):
        return P.op("scalar", name, r, w, **kw)

    def GP(name, r, w, *a, **kw):
        return P.op("gpsimd", name, r, w, *a, **kw)

    def MM(r, w, out, lhsT, rhs, start=True, stop=True):
        return P.op("tensor", "matmul", r, w, out, lhsT=lhsT, rhs=rhs, start=start, stop=stop)

    def TR(r, w, out, in_, ident):
        return P.op("tensor", "transpose", r, w, out=out, in_=in_, identity=ident)

    def ACT(r, w, out, in_, func, **kw):
        return P.op("scalar", "activation", r, w, out=out, in_=in_, func=func, **kw)

    xs = din("xs", [NTOK, D])
    xp = din("xp", [max(NPRE, 128), D])
    flag = din("flag", [128, 1])
    cT = din("cT", [128, 8])
    ada_w = din("ada_w", [D, 6 * D])
    ada_bT = din("ada_bT", [128, 48])
    abg = din("abg", [128, 2, D])
    n1gT = din("n1gT", [128, 8])
    n2gT = din("n2gT", [128, 8])
    w_in = din("w_in", [D, 3072])
    w_out = din("w_out", [D, D])
    wq = din("wq", [D, 2048])
    lbg = din("lbg", [128, 2, 4])
    vec512 = din("vec512", [128, 4, 512])
    fg_d = din("fg_bc", [128, D])
    spw = din("spw", [4, 128, 128])
    spbT = din("spbT", [128, 4])
    keys = din("keys", [16, 128, 128])
    pu = din("pu", [128 * NEXP_CH, D])
    pv = din("pv", [128 * NEXP_CH, D])
    out_d = nc.dram_tensor("out", [NTOK, D], F32, kind="ExternalOutput").ap()
    win_s = nc.dram_tensor("win_s", [128, 8, 3072], BF16, kind="Internal").ap()
    wout_s = nc.dram_tensor("wout_s", [128, 8, 1024], BF16, kind="Internal").ap()
    wq_s = nc.dram_tensor("wq_s", [128, 8, 2048], BF16, kind="Internal").ap()
    uv_s = nc.dram_tensor("uv_s", [NEXP_CH, 128, 2048], BF16, kind="Internal").ap()
    dbg_d = None
    dbg2_d = None
    if dbg:
        dbg_d = nc.dram_tensor("dbg", [NTOK, D], F32, kind="ExternalOutput").ap()
        dbg2_d = nc.dram_tensor("dbg2", [NTOK, D], BF16, kind="ExternalOutput").ap()

    BK = [Buf(es.enter_context(nc.psum_tensor("bk%d" % i, [128, 512], F32)), "bk%d" % i)
          for i in range(8)]

    def bkb(i):
        return BK[i].ap[:].bitcast(BF16)

    OVK = ["ov%d" % r for r in range(32)]
    OV = sb("ov", [128, 32768], BF16, key=OVK)
    H = [OVK[0:16], OVK[16:32]]
    def wk(kc):
        return OVK[3 * kc:3 * kc + 3]
    ov_f32 = OV.ap[:].bitcast(F32)
    w_in_v = OV.ap[:, 0:24576].rearrange("p (c n) -> p c n", n=3072)
    w_out_v = OV.ap[:, 24576:32768].rearrange("p (c n) -> p c n", n=1024)
    G_v = OV.ap[:].rearrange("p (i t) -> p i t", t=TB)

    ident = sb("ident", [128, 128])
    identb = sb("identb", [128, 128], BF16)
    iotab = sb("iotab", [128, 128], BF16)
    iota16 = sb("iota16", [128, 16])
    maskT = sb("maskT", [128, 128])
    scanm = sb("scanm", [128, 512])
    cact = sb("cact", [128, 8])
    modT = sb("modT", [128, 4, 8])
    QG = sb("qg", [128, 2 * D])
    gate_bc = Buf(QG.ap[:].rearrange("p (a n) -> p a n", n=D), "qg")
    A1 = sb("A1", [128, 8]); A2 = sb("A2", [128, 8])
    smallin = sb("smallin", [128, 80])
    abT = sb("abT", [128, 48])
    lbt = sb("lbt", [128, 4]); l1m = sb("l1m", [128, 4]); lbtmp = sb("lbtmp", [128, 4])
    vecs = sb("vecs", [128, 4, 512])
    fgb = sb("fgb", [128, D])
    keysT = sb("keysT", [128, 16, 128], BF16)
    WT = sb("WT", [128, 4, 128], BF16)
    S32 = sb("S32", [128, 4, 128])
    SbA = sb("SbA", [128, 4, 128], BF16)
    SbB = sb("SbB", [128, 4, 128], BF16)

    xt = [sb("xt%d" % i, [128, D]) for i in range(2)]
    x1 = [sb("x1_%d" % i, [128, D]) for i in range(2)]
    junk = sb("junk", [128, D], BF16)
    xsb = sb("xsb", [128, D], BF16)
    st4 = sb("st4", [128, 16])
    hT = sb("hT", [128, 8, 128], BF16)

    h2T = sb("h2T", [128, 8, TB], BF16)
    T5 = sb("t5", [128, 6, 512])
    t512 = [Buf(T5.ap[:, i, :], "t5_%d" % i) for i in range(6)]
    kTt = sb("kTt", [128, 4, 128], BF16)
    qTt = sb("qTt", [128, 4, 128], BF16)
    qA = sb("qA", [128, 4, 128], BF16)
    qB = sb("qB", [128, 4, 128], BF16)
    ebl = sb("ebl", [128, 4, 2])
    vb = sb("vb", [128, 512], BF16)
    scm = sb("scm", [128, 4, 128], BF16)
    ktok = sb("ktok", [128, 4, 128], BF16)
    cat = sb("cat", [128, D], BF16)
    catT = sb("catT", [128, 8, 128], BF16)
    hT2 = Buf(catT.ap, "catT")
    vlnb = sb("vlnb", [128, 512], BF16)
    qTs = Buf(QG.ap[:].bitcast(BF16).rearrange("p (g t) -> p g t", t=TB), "qg")
    TK = sb("tk", [128, 4096])
    sim = Buf(TK.ap[:, 0:2048].rearrange("p (g n) -> p g n", n=128), "tk0")
    simw = Buf(TK.ap[:, 2048:4096].rearrange("p (g n) -> p g n", n=128), "tk1")
    cand = Buf(TK.ap[:, 0:2048].rearrange("p (h c) -> p h c", c=256), "tk0")
    candw = Buf(TK.ap[:, 2048:4096].rearrange("p (h c) -> p h c", c=256), "tk1")
    crep = Buf(TK.ap[:, 0:1024].rearrange("p (c m) -> p c m", m=128), "tk0")
    s16 = sb("s16", [128, 16, 16])
    i16 = sb("i16", [128, 16, 16], U32)
    i16f = sb("i16f", [128, 16, 16])
    ts16 = sb("ts16", [128, 8, 16])
    pos = sb("pos", [128, 8, 16], U32)
    pa_u = sb("pa_u", [128, 8, 16], U32); pb_u = sb("pb_u", [128, 8, 16], U32)
    pa_f = sb("pa_f", [128, 8, 16]); pb_f = sb("pb_f", [128, 8, 16])
    gexp = sb("gexp", [128, 8, 16]); gate = sb("gate", [128, 8, 16])
    isel = sb("isel", [128, 8, 16]); jsel = sb("jsel", [128, 8, 16])
    kI = sb("kI", [128, TB], BF16); kJ = sb("kJ", [128, TB], BF16); kG = sb("kG", [128, TB], BF16)
    Pm = [sb("Pm%d" % i, [128, 128], BF16) for i in range(4)]
    Qm = [sb("Qm%d" % i, [128, 128], BF16) for i in range(4)]
    NU = 6
    NV = 5
    UV = sb("uv", [128, NU + NV, 1024], BF16)
    UTs = [Buf(UV.ap[:, i, :], "UTs%d" % i) for i in range(NU)]
    VBs = [Buf(UV.ap[:, NU + i, :], "VBs%d" % i) for i in range(NV)]
    wqs = [Buf(UV.ap[:, 0:2, :].rearrange("p a (c n) -> p (a c) n", n=256), ["UTs0", "UTs1"]),
           Buf(UV.ap[:, NU:NU + 2, :].rearrange("p a (c n) -> p (a c) n", n=256), ["VBs0", "VBs1"])]
    ga = [sb("ga%d" % i, [128, TB]) for i in range(3)]
    Wt = [sb("Wt%d" % i, [128, TB], BF16) for i in range(3)]
    stg = [Buf(T5.ap[:, 2:4, :].rearrange("p a n -> p (a n)"), ["t5_2", "t5_3"]),
           Buf(T5.ap[:, 4:6, :].rearrange("p a n -> p (a n)"), ["t5_4", "t5_5"])]
    print("sbuf bytes/partition left:", nc.sbuf_bytes_remaining)

    def rstd_from_ss(ss_ap, n, out_ap, r, w):
        ACT(r, w, out_ap, ss_ap, AF.Ln, scale=1.0 / n, bias=EPS)
        ACT(w, w, out_ap, out_ap, AF.Exp, scale=-0.5)

    GP("memset", [], [ident], ident[:], 1.0)
    GP("affine_select", [ident], [ident], out=ident[:], in_=ident[:], pattern=[[-1, 128]],
       compare_op=ALU.is_equal, fill=0.0, base=0, channel_multiplier=1)
    V("tensor_copy", [ident], [identb], out=identb[:], in_=ident[:])
    GP("iota", [], [maskT], maskT[:], pattern=[[1, 128]], base=0, channel_multiplier=0,
       allow_small_or_imprecise_dtypes=True)
    V("tensor_copy", [maskT], [iotab], out=iotab[:], in_=maskT[:])
    V("tensor_copy", [maskT], [iota16], out=iota16[:], in_=maskT[:, 0:16])
    GP("memset", [iotab, iota16], [maskT], maskT[:], 1.0)
    GP("affine_select", [maskT], [maskT], out=maskT[:], in_=maskT[:], pattern=[[1, 128]],
       compare_op=ALU.is_ge, fill=0.0, base=0, channel_multiplier=-1)
    GP("memset", [maskT], [maskT], maskT[0:64, 64:128], 0.0)
    GP("memset", [], [scanm], scanm[:], 1.0)
    for c in range(8):
        GP("memset", [scanm], [scanm], scanm[:, c * 64:c * 64 + 1], 0.0)
    GP("memset", [], [S32], S32[:], 0.0)
    GP("memset", [], [SbA], SbA[:], 0.0)
    GP("memset", [], [qA], qA[:], 0.0)
    GP("memset", [], [qB], qB[:], 0.0)

    P.dma("sm", [], [smallin], smallin[:, 0:8], cT[:, :])
    P.dma("sm", [], [smallin], smallin[:, 8:16], n1gT[:, :])
    P.dma("sm", [], [smallin], smallin[:, 16:24], n2gT[:, :])
    P.dma("sm", [], [smallin], smallin[:, 24:32], lbg[:, :, :].rearrange("p a b -> p (a b)"))
    P.dma("sm", [], [smallin], smallin[:, 32:36], spbT[:, :])
    P.dma("sm", [], [smallin], smallin[:, 36:37], flag[:, :])
    P.dma("abT", [], [abT], abT[:], ada_bT[:, :])
    P.dma("gatebc", [], [gate_bc], gate_bc[:], abg[:, :, :])
    P.dma("vecs", [], [vecs], vecs[:], vec512[:, :, :])
    P.dma("fgb", [], [fgb], fgb[:], fg_d[:, :])
    cTs = smallin.ap[:, 0:8]; n1g = smallin.ap[:, 8:16]; n2g = smallin.ap[:, 16:24]
    lbg_s = smallin.ap[:, 24:32]; spb = smallin.ap[:, 32:36]; flg = smallin.ap[:, 36:37]
    hg_bc = vecs.ap[:, 0, :]; lng_bc = vecs.ap[:, 1, :]; lnb_bc = vecs.ap[:, 2, :]; gng_bc = vecs.ap[:, 3, :]

    V("tensor_tensor", [smallin], [lbtmp], out=lbtmp[:], in0=lbg_s[:, 4:8], in1=lbg_s[:, 0:4], op=ALU.subtract)
    ACT([lbtmp], [lbt], lbt[:], lbtmp[:], AF.Exp)
    ACT([lbt], [l1m], l1m[:], lbt[:], AF.Ln, bias=1.0)
    V("tensor_tensor", [lbtmp, l1m], [l1m], out=l1m[:], in0=lbtmp[:], in1=l1m[:], op=ALU.subtract)
    V("tensor_scalar", [lbt], [lbt], out=lbt[:], in0=lbt[:], scalar1=1.0, scalar2=None, op0=ALU.add)
    V("reciprocal", [lbt], [lbt], out=lbt[:], in_=lbt[:])

    ACT([smallin], [cact], cact[:], cTs, AF.Exp, scale=-1.0)
    ACT([cact], [cact], cact[:], cact[:], AF.Ln, bias=1.0)
    ACT([cact], [cact], cact[:], cact[:], AF.Exp, scale=-1.0)
    V("tensor_tensor", [cact, smallin], [cact], out=cact[:], in0=cact[:], in1=cTs, op=ALU.mult)
    for kc in range(8):
        V("tensor_copy", [cact], [crep], out=crep[:, kc, :], in_=cact[:, kc:kc + 1].to_broadcast([128, 128]))

    ada_v = ada_w.rearrange("(c p) n -> p c n", p=128)
    for s in range(6):
        half = s % 2
        stage = ov_f32[:, half * 8192:(half + 1) * 8192].rearrange("p (c n) -> p c n", n=1024)
        key = H[half]
        for kc in range(8):
            P.dma("ovh%d" % half, [], [key], stage[:, kc, :], ada_v[:, kc, s * 1024:(s + 1) * 1024])
        if s in (2, 5):
            gi = 0 if s == 2 else 1
            for hf in range(2):
                for kc in range(8):
                    MM([crep, key], [BK[hf]], BK[hf][:, :], lhsT=crep[:, kc, :],
                       rhs=stage[:, kc, hf * 512:(hf + 1) * 512], start=(kc == 0), stop=(kc == 7))
                V("tensor_tensor", [BK[hf], gate_bc], [gate_bc], out=gate_bc[:, gi, hf * 512:(hf + 1) * 512],
                  in0=BK[hf][:, :], in1=gate_bc[:, gi, hf * 512:(hf + 1) * 512], op=ALU.add)
        else:
            mi = {0: 0, 1: 1, 3: 2, 4: 3}[s]
            for cc in range(8):
                for kc in range(8):
                    MM([cact, key], [BK[2]], BK[2][:, cc:cc + 1], lhsT=stage[:, kc, cc * 128:(cc + 1) * 128],
                       rhs=cact[:, kc:kc + 1], start=(kc == 0), stop=(kc == 7))
            V("tensor_tensor", [BK[2], abT], [modT], out=modT[:, mi, :], in0=BK[2][:, 0:8],
              in1=abT[:, s * 8:(s + 1) * 8], op=ALU.add)
    V("scalar_tensor_tensor", [modT, smallin], [A1], out=A1[:], in0=modT[:, 1, :], scalar=1.0, in1=n1g,
      op0=ALU.add, op1=ALU.mult)
    V("scalar_tensor_tensor", [modT, smallin], [A2], out=A2[:], in0=modT[:, 3, :], scalar=1.0, in1=n2g,
      op0=ALU.add, op1=ALU.mult)
    B1 = modT.ap[:, 0, :]
    B2 = modT.ap[:, 2, :]

    kst = ov_f32[:, 0:2048].rearrange("p (g d) -> p g d", d=128)
    P.dma("ovh0", [], [H[0]], kst, keys.rearrange("g n d -> n g d"))
    for g in range(16):
        b = 3 + (g % 2)
        TR([H[0], ident], [BK[b]], BK[b][:, 0:128], kst[:, g, :], ident[:])
        V("tensor_copy", [BK[b]], [keysT], out=keysT[:, g, :], in_=BK[b][:, 0:128])
    wst = ov_f32[:, 8192:8192 + 512].rearrange("p (g d) -> p g d", d=128)
    P.dma("ovh1", [], [H[1]], wst, spw.rearrange("g t s -> t g s"))
    for g in range(4):
        GP("affine_select", [H[1]], [H[1]], out=wst[:, g, :], in_=wst[:, g, :], pattern=[[-1, 128]],
           compare_op=ALU.is_ge, fill=0.0, base=0, channel_multiplier=1)
        b = 3 + (g % 2)
        TR([H[1], ident], [BK[b]], BK[b][:, 0:128], wst[:, g, :], ident[:])
        V("tensor_copy", [BK[b]], [WT], out=WT[:, g, :], in_=BK[b][:, 0:128])

    def prep_weight(src, ncols, dst, dkey, scale_bc=None):
        sv = src.rearrange("(c p) n -> p c n", p=128)
        for j in range(ncols // 512):
            half = j % 2
            key = H[half]
            slot = "ovh%d" % half
            stage = ov_f32[:, half * 8192:half * 8192 + 4096].rearrange("p (c n) -> p c n", n=512)
            stb = OV.ap[:, half * 16384 + 8192: half * 16384 + 8192 + 4096].rearrange("p (c n) -> p c n", n=512)
            P.dma(slot, [], [key], stage, sv[:, :, j * 512:(j + 1) * 512])
            if scale_bc is None:
                if j % 2 == 0:
                    V("tensor_copy", [key], [key], out=stb, in_=stage)
                else:
                    GP("tensor_copy", [key], [key], out=stb, in_=stage)
            else:
                V("tensor_tensor", [key, gate_bc], [key], out=stb, in0=stage,
                  in1=scale_bc[:, j * 512:(j + 1) * 512].unsqueeze(1).to_broadcast([128, 8, 512]), op=ALU.mult)
            P.dma(slot, [key], [(dkey, j)], dst[:, :, j * 512:(j + 1) * 512], stb)

    prep_weight(w_in, 3072, win_s, "win_s")
    prep_weight(w_out, 1024, wout_s, "wout_s", scale_bc=gate_bc.ap[:, 0, :])
    prep_weight(wq, 2048, wq_s, "wq_s")

    pu_v = pu.rearrange("(i j) d -> i j d", j=128)
    pv_v = pv.rearrange("(i j) d -> i j d", j=128)
    Ust = [Buf(ov_f32[:, 12288 + sl * 1024:12288 + (sl + 1) * 1024], [OVK[24 + 2 * sl], OVK[25 + 2 * sl]]) for sl in range(2)]
    Vst = [Buf(ov_f32[:, 14336 + sl * 1024:14336 + (sl + 1) * 1024], [OVK[28 + 2 * sl], OVK[29 + 2 * sl]]) for sl in range(2)]

    def prep_load(i):
        sl = i % 2
        P.dma("ust%d" % sl, [], [Ust[sl]], Ust[sl][:], pu_v[i])
        P.dma("vst%d" % sl, [], [Vst[sl]], Vst[sl][:], pv_v[i])

    def prep_chunk(i):
        sl = i % 2
        if i + 1 < NEXP_CH:
            prep_load(i + 1)
        us = UTs[i % NU]
        for hb in range(2):
            b = 6 + hb
            for q in range(4):
                dc = hb * 4 + q
                TR([Ust[sl], ident], [BK[b]], BK[b][:, q * 128:(q + 1) * 128], Ust[sl][:, dc * 128:(dc + 1) * 128], ident[:])
            if hb == 0:
                V("tensor_copy", [BK[b]], [us], out=us[:, 0:512], in_=BK[b][:, :])
            else:
                S("copy", [BK[b]], [us], out=us[:, 512:1024], in_=BK[b][:, :])
        P.dma("UTs%d" % (i % NU), [us], [("ut", i)], uv_s[i][:, 0:1024], us[:])
        vs = VBs[i % NV]
        GP("tensor_tensor", [Vst[sl], gate_bc], [vs], out=vs[:], in0=Vst[sl][:], in1=gate_bc[:, 1, :], op=ALU.mult)
        P.dma("VBs%d" % (i % NV), [vs], [("vb", i)], uv_s[i][:, 1024:2048], vs[:])

    def load_win_piece(pc):
        if pc < 8:
            P.dma("ovr%d" % pc, [("win_s", j) for j in range(6)], [wk(pc)], w_in_v[:, pc, :], win_s[:, pc, :])
        else:
            cc = pc - 8
            P.dma("ovr%d" % pc, [("wout_s", j) for j in range(2)], [OVK[24 + cc]], w_out_v[:, cc, :], wout_s[:, cc, :])

    def load_win():
        for pc in range(16):
            load_win_piece(pc)

    h2T_t = [Buf(h2T.ap, "h2T0"), Buf(h2T.ap, "h2T1")]
    st5 = sb("st5", [128, 16])

    def run(gen):
        for _ in gen:
            pass

    def sched(streams):
        done = set()
        pending = list(streams)
        active = []
        while pending or active:
            for st in list(pending):
                if all(d in done for d in st[2]):
                    pending.remove(st)
                    active.append((st[0], st[1]()))
            for ent in list(active):
                try:
                    next(ent[1])
                except StopIteration:
                    active.remove(ent)
                    done.add(ent[0])

    def interleave(ga_, gb_, wa=1, wb=1):
        gens = [[ga_, wa], [gb_, wb]]
        while gens:
            for ent in list(gens):
                for _ in range(ent[1]):
                    try:
                        next(ent[0])
                    except StopIteration:
                        gens.remove(ent)
                        break

    def norm_transpose(src, A, Bv, dstT, col0, bank):
        ACT([src], [junk, st4], junk[:], src[:], AF.Square, accum_out=st4[:, 0:1])
        rstd_from_ss(st4[:, 0:1], D, st4[:, 1:2], [st4], [st4])
        V("tensor_scalar", [src, st4], [xsb], out=xsb[:], in0=src[:], scalar1=st4[:, 1:2], scalar2=None, op0=ALU.mult)
        yield
        pt = bkb(bank)
        for dc in range(8):
            TR([xsb, identb], [BK[bank]], pt[:, dc * 128:(dc + 1) * 128], xsb[:, dc * 128:(dc + 1) * 128], identb[:])
        yield
        for dc in range(8):
            ACT([BK[bank], A, modT], [dstT], dstT[:, dc, col0:col0 + 128], pt[:, dc * 128:(dc + 1) * 128],
                AF.Identity, scale=A[:, dc:dc + 1], bias=Bv[:, dc:dc + 1])
        yield

    def hgrn_state_and_scores(full, hT=hT):
        E, sp, l1, zsp, bb = t512[0:5]
        arg = zsp; Eq = E; spq = sp
        pZ = BK[2]; pQ = BK[1]; pV = BK[3]
        for h in range(4):
            for kc in range(8):
                MM([wk(kc), hT], [pZ], pZ[:, h * 128:(h + 1) * 128], lhsT=w_in_v[:, kc, 512 + h * 128:512 + (h + 1) * 128],
                   rhs=hT[:, kc, :], start=(kc == 0), stop=(kc == 7))
            if h % 2 == 1:
                yield
        for kc in range(8):
            MM([wk(kc), hT], [pV], pV[:, :], lhsT=hT[:, kc, :], rhs=w_in_v[:, kc, 1024:1536], start=(kc == 0), stop=(kc == 7))
        yield
        if full:
            for h in range(4):
                for kc in range(8):
                    MM([wk(kc), hT], [pQ], pQ[:, h * 128:(h + 1) * 128], lhsT=w_in_v[:, kc, h * 128:(h + 1) * 128],
                       rhs=hT[:, kc, :], start=(kc == 0), stop=(kc == 7))
                if h % 2 == 1:
                    yield
        ACT([pZ], [E], E[:], pZ[:, :], AF.Exp, scale=-1.0)
        ACT([E], [sp], sp[:], E[:], AF.Ln, bias=1.0)
        for h in range(4):
            ACT([E, lbt], [l1], l1[:, h * 128:(h + 1) * 128], E[:, h * 128:(h + 1) * 128], AF.Ln,
                scale=lbt[:, h:h + 1], bias=1.0)
        S("copy", [pV], [vb], out=vb[:], in_=pV[:, :])
        yield
        V("tensor_tensor", [pZ, sp], [zsp], out=zsp[:], in0=pZ[:, :], in1=sp[:], op=ALU.add)
        V("tensor_tensor", [l1, sp], [l1], out=l1[:], in0=l1[:], in1=sp[:], op=ALU.subtract)
        V("tensor_tensor_scan", [scanm, l1], [bb], out=bb[:], data0=scanm[:], data1=l1[:], initial=0.0,
          op0=ALU.mult, op1=ALU.add)
        V("tensor_tensor", [zsp, bb], [arg], out=arg[:], in0=zsp[:], in1=bb[:], op=ALU.add)
        yield
        for h in range(4):
            ACT([arg, l1m], [kTt], kTt[:, h, :], arg[:, h * 128:(h + 1) * 128], AF.Exp, scale=-1.0,
                bias=l1m[:, h:h + 1])
        bb3 = bb.ap[:].rearrange("p (h t) -> p h t", t=128)
        ACT([bb], [ebl], ebl[:], bb3[:, :, 63:128:64], AF.Exp)
        yield
        if full:
            ACT([pQ], [Eq], Eq[:], pQ[:, :], AF.Exp, scale=-1.0)
            ACT([Eq], [spq], spq[:], Eq[:], AF.Ln, bias=1.0)
            V("tensor_tensor", [bb, spq], [spq], out=spq[:], in0=bb[:], in1=spq[:], op=ALU.subtract)
            ACT([spq], [Eq], Eq[:], spq[:], AF.Exp)
            V("tensor_tensor", [pQ, Eq], [qTt], out=qTt[:].rearrange("p h t -> p (h t)"), in0=pQ[:, :], in1=Eq[:], op=ALU.mult)
            GP("tensor_copy", [qTt], [qA], out=qA[:, :, 0:64], in_=qTt[:, :, 0:64])
            GP("tensor_copy", [qTt], [qB], out=qB[:, :, 64:128], in_=qTt[:, :, 64:128])
            yield
        pK = bkb(0)
        for h in range(4):
            TR([kTt, identb], [BK[0]], pK[:, h * 128:(h + 1) * 128], kTt[:, h, :], identb[:])
        S("copy", [BK[0]], [ktok], out=ktok[:].rearrange("p h k -> p (h k)"), in_=pK[:, 0:512])
        yield
        pO = BK[2]; pS = BK[1]
        if full:
            for h in range(4):
                MM([kTt, qTt], [pS], pS[:, h * 128:(h + 1) * 128], lhsT=kTt[:, h, :], rhs=qTt[:, h, :])
            V("tensor_tensor", [pS, maskT], [scm], out=scm[:], in0=pS[:, :].rearrange("p (h t) -> p h t", t=128),
              in1=maskT[:].unsqueeze(1).to_broadcast([128, 4, 128]), op=ALU.mult)
            yield
        pSt = BK[5]
        for c in range(2):
            lo = c * 64
            for h in range(4):
                MM([ktok, vb], [pSt], pSt[:, h * 128:(h + 1) * 128], lhsT=ktok[lo:lo + 64, h, :],
                   rhs=vb[lo:lo + 64, h * 128:(h + 1) * 128])
            V("tensor_tensor", [S32, pSt], [S32], out=S32[:].rearrange("p h v -> p (h v)"),
              in0=S32[:].rearrange("p h v -> p (h v)"), in1=pSt[:, :], op=ALU.add)
            V("tensor_tensor", [S32, ebl], [S32], out=S32[:], in0=S32[:],
              in1=ebl[:, :, c:c + 1].to_broadcast([128, 4, 128]), op=ALU.mult)
            dst = SbB if c == 0 else SbA
            if c == 0 and full:
                S("copy", [S32], [dst], out=dst[:], in_=S32[:])
                yield
                for h in range(4):
                    MM([scm, vb], [pO], pO[:, h * 128:(h + 1) * 128], lhsT=scm[:, h, :], rhs=vb[:, h * 128:(h + 1) * 128],
                       start=True, stop=False)
                    MM([qA, SbA], [pO], pO[:, h * 128:(h + 1) * 128], lhsT=qA[:, h, :], rhs=SbA[:, h, :],
                       start=False, stop=False)
                    MM([qB, SbB], [pO], pO[:, h * 128:(h + 1) * 128], lhsT=qB[:, h, :], rhs=SbB[:, h, :],
                       start=False, stop=True)
            elif full:
                GP("tensor_copy", [S32], [dst], out=dst[:], in_=S32[:])
            yield

    st6 = sb("st6", [128, 16])

    def par(*gens_):
        gl = list(gens_)
        while gl:
            for g_ in list(gl):
                try:
                    next(g_)
                except StopIteration:
                    gl.remove(g_)
            yield

    def mixer_H():
        yield from hgrn_state_and_scores(True)
        pO = BK[2]
        a0 = t512[0]
        pOG = BK[3]
        for kc in range(8):
            MM([wk(kc), hT], [pOG], pOG[:, :], lhsT=hT[:, kc, :], rhs=w_in_v[:, kc, 1536:2048], start=(kc == 0), stop=(kc == 7))
        yield
        for h in range(4):
            ACT([pO], [junk, st4], junk[:, 0:128], pO[:, h * 128:(h + 1) * 128], AF.Square, accum_out=st4[:, 4 + h:5 + h])
        rstd_from_ss(st4[:, 4:8], 128, st4[:, 8:12], [st4], [st4])
        yield
        ACT([pOG], [a0], a0[:], pOG[:, :], AF.Exp, scale=-1.0)
        ACT([a0], [a0], a0[:], a0[:], AF.Ln, bias=1.0)
        ACT([a0], [a0], a0[:], a0[:], AF.Exp, scale=-1.0)
        V("tensor_tensor", [pOG, a0], [a0], out=a0[:], in0=pOG[:, :], in1=a0[:], op=ALU.mult)
        V("tensor_tensor", [a0, vecs], [a0], out=a0[:], in0=a0[:], in1=hg_bc, op=ALU.mult)
        yield
        for h in range(4):
            V("scalar_tensor_tensor", [pO, st4, a0], [cat], out=cat[:, h * 128:(h + 1) * 128],
              in0=pO[:, h * 128:(h + 1) * 128], scalar=st4[:, 8 + h:9 + h], in1=a0[:, h * 128:(h + 1) * 128],
              op0=ALU.mult, op1=ALU.mult)
        yield

    def mixer_M(x1o):
        a1 = Buf(x1o.ap[:, 0:512], x1o.key)
        a2 = Buf(x1o.ap[:, 512:1024], x1o.key)
        a3 = t512[5]
        pU = BK[4]; pVG = BK[4]; pM = BK[4]
        for kc in range(8):
            MM([wk(kc), hT], [pU], pU[:, :], lhsT=hT[:, kc, :], rhs=w_in_v[:, kc, 2048:2560], start=(kc == 0), stop=(kc == 7))
        yield
        ACT([pU], [a1], a1[:], pU[:, :], AF.Gelu_apprx_tanh)
        for kc in range(8):
            MM([wk(kc), hT], [pVG], pVG[:, :], lhsT=hT[:, kc, :], rhs=w_in_v[:, kc, 2560:3072], start=(kc == 0), stop=(kc == 7))
        yield
        ACT([pVG], [a2], a2[:], pVG[:, :], AF.Gelu_apprx_tanh)
        V("tensor_reduce", [a2], [st6], out=st6[:, 2:3], in_=a2[:], axis=AX.X, op=ALU.add)
        V("tensor_scalar", [st6], [st6], out=st6[:, 3:4], in0=st6[:, 2:3], scalar1=1.0 / 512, scalar2=None, op0=ALU.mult)
        V("tensor_scalar", [a2, st6], [a2], out=a2[:], in0=a2[:], scalar1=st6[:, 3:4], scalar2=None, op0=ALU.subtract)
        yield
        ACT([a2], [junk, st6], junk[:, 512:1024], a2[:], AF.Square, accum_out=st6[:, 12:13])
        rstd_from_ss(st6[:, 12:13], 512, st6[:, 13:14], [st6], [st6])
        V("scalar_tensor_tensor", [a2, st6, vecs], [a2], out=a2[:], in0=a2[:], scalar=st6[:, 13:14], in1=lng_bc,
          op0=ALU.mult, op1=ALU.mult)
        V("tensor_tensor", [a2, vecs], [vlnb], out=vlnb[:], in0=a2[:], in1=lnb_bc, op=ALU.add)
        yield
        for g in range(4):
            MM([WT, vlnb], [pM], pM[:, g * 128:(g + 1) * 128], lhsT=WT[:, g, :], rhs=vlnb[:, g * 128:(g + 1) * 128])
        for g in range(4):
            V("scalar_tensor_tensor", [pM, smallin, a1], [a3], out=a3[:, g * 128:(g + 1) * 128],
              in0=pM[:, g * 128:(g + 1) * 128], scalar=spb[:, g:g + 1], in1=a1[:, g * 128:(g + 1) * 128],
              op0=ALU.add, op1=ALU.mult)
        yield
        for g in range(4):
            ACT([a3], [junk, st6], junk[:, 512:640], a3[:, g * 128:(g + 1) * 128], AF.Square, accum_out=st6[:, 4 + g:5 + g])
        rstd_from_ss(st6[:, 4:8], 128, st6[:, 8:12], [st6], [st6])
        for g in range(4):
            V("scalar_tensor_tensor", [a3, st6, vecs], [cat], out=cat[:, 512 + g * 128:512 + (g + 1) * 128],
              in0=a3[:, g * 128:(g + 1) * 128], scalar=st6[:, 8 + g:9 + g], in1=gng_bc[:, g * 128:(g + 1) * 128],
              op0=ALU.mult, op1=ALU.mult)
        yield

    def mixer_tile(xin, x1o, tt, drow):
        yield from norm_transpose(xin, A1, B1, hT, 0, 0)
        yield from par(mixer_H(), mixer_M(x1o))
        if dbg:
            P.dma("dbg2", [cat], [], dbg2_d[drow:drow + 128, :], cat[:])
        pt = bkb(0)
        for cc in range(8):
            TR([cat, identb], [BK[0]], pt[:, cc * 128:(cc + 1) * 128], cat[:, cc * 128:(cc + 1) * 128], identb[:])
        S("copy", [BK[0]], [catT], out=catT[:].rearrange("p c t -> p (c t)"), in_=pt[:, :])
        yield
        for hf in range(2):
            b = (5, 1)[hf]
            for cc in range(8):
                MM([OVK[24 + cc], catT], [BK[b]], BK[b][:, :], lhsT=catT[:, cc, :], rhs=w_out_v[:, cc, hf * 512:(hf + 1) * 512],
                   start=(cc == 0), stop=(cc == 7))
            V("tensor_tensor", [xin, BK[b]], [x1o], out=x1o[:, hf * 512:(hf + 1) * 512],
              in0=xin[:, hf * 512:(hf + 1) * 512], in1=BK[b][:, :], op=ALU.add)
            yield
        yield from norm_transpose(x1o, A2, B2, h2T_t[tt], tt * 128, 0)

    def peer_q(tt):
        c0 = tt * 128
        for g2 in range(8):
            ws = wqs[g2 % 2]
            P.dma("wqs%d" % (g2 % 2), [("wq_s", j) for j in range(4)], [ws], ws[:], wq_s[:, :, g2 * 256:(g2 + 1) * 256])
            for gg in range(2):
                g = g2 * 2 + gg
                b = 6
                for kc in range(8):
                    MM([ws, h2T_t[tt]], [BK[b]], BK[b][:, 0:128], lhsT=ws[:, kc, gg * 128:(gg + 1) * 128],
                       rhs=h2T[:, kc, c0:c0 + 128], start=(kc == 0), stop=(kc == 7))
                S("copy", [BK[b]], [qTs], out=qTs[:, g, c0:c0 + 128], in_=BK[b][:, 0:128])
            yield

    def peer_topk(tt):
        c0 = tt * 128
        for qd in range(4):
            b = 7
            for j in range(4):
                g = qd * 4 + j
                MM([qTs, keysT], [BK[b]], BK[b][:, j * 128:(j + 1) * 128], lhsT=qTs[:, g, c0:c0 + 128], rhs=keysT[:, g, :])
            S("copy", [BK[b]], [sim], out=sim[:, qd * 4:(qd + 1) * 4, :].rearrange("p g n -> p (g n)"), in_=BK[b][:, :])
            yield
        for g in range(16):
            V("max", [sim], [s16], out=s16[:, g, 0:8], in_=sim[:, g, :])
            if g % 4 == 3:
                yield
        for g in range(16):
            V("max_index", [sim, s16], [i16], out=i16[:, g, 0:8], in_max=s16[:, g, 0:8], in_values=sim[:, g, :])
            if g % 4 == 3:
                yield
        for g in range(16):
            V("match_replace", [sim, s16], [simw], out=simw[:, g, :], in_to_replace=s16[:, g, 0:8],
              in_values=sim[:, g, :], imm_value=-1e30)
            if g % 4 == 3:
                yield
        for g in range(16):
            V("max", [simw], [s16], out=s16[:, g, 8:16], in_=simw[:, g, :])
            if g % 4 == 3:
                yield
        for g in range(16):
            V("max_index", [simw, s16], [i16], out=i16[:, g, 8:16], in_max=s16[:, g, 8:16], in_values=simw[:, g, :])
            if g % 4 == 3:
                yield
        V("tensor_copy", [i16], [i16f], out=i16f[:], in_=i16[:])
        s16v = s16.ap[:].rearrange("p (h q) a -> p h q a", q=2)
        GP("tensor_tensor", [s16], [cand], out=cand[:].rearrange("p h (a b) -> p h a b", b=16),
          in0=s16v[:, :, 0, :].unsqueeze(3).to_broadcast([128, 8, 16, 16]),
          in1=s16v[:, :, 1, :].unsqueeze(2).to_broadcast([128, 8, 16, 16]), op=ALU.add)
        yield
        for h in range(8):
            V("max", [cand], [ts16], out=ts16[:, h, 0:8], in_=cand[:, h, :])
            if h == 3:
                yield
        yield
        for h in range(8):
            V("max_index", [cand, ts16], [pos], out=pos[:, h, 0:8], in_max=ts16[:, h, 0:8], in_values=cand[:, h, :])
        yield
        for h in range(8):
            V("match_replace", [cand, ts16], [candw], out=candw[:, h, :], in_to_replace=ts16[:, h, 0:8],
              in_values=cand[:, h, :], imm_value=-1e30)
        yield
        for h in range(8):
            V("max", [candw], [ts16], out=ts16[:, h, 8:16], in_=candw[:, h, :])
            if h == 3:
                yield
        yield
        for h in range(8):
            V("max_index", [candw, ts16], [pos], out=pos[:, h, 8:16], in_max=ts16[:, h, 8:16], in_values=candw[:, h, :])
        yield
        V("tensor_tensor", [ts16], [gexp], out=gexp[:], in0=ts16[:], in1=ts16[:, :, 0:1].to_broadcast([128, 8, 16]),
          op=ALU.subtract)
        ACT([gexp], [gexp], gexp[:], gexp[:], AF.Exp)
        V("tensor_reduce", [gexp], [st5], out=st5[:, 8:16], in_=gexp[:], axis=AX.X, op=ALU.add)
        V("reciprocal", [st5], [st5], out=st5[:, 8:16], in_=st5[:, 8:16])
        V("tensor_tensor", [gexp, st5], [gate], out=gate[:], in0=gexp[:],
          in1=st5[:, 8:16].unsqueeze(2).to_broadcast([128, 8, 16]), op=ALU.mult)
        yield
        V("tensor_single_scalar", [pos], [pa_u], out=pa_u[:], in_=pos[:], scalar=4, op=ALU.logical_shift_right)
        V("tensor_single_scalar", [pos], [pb_u], out=pb_u[:], in_=pos[:], scalar=15, op=ALU.bitwise_and)
        V("tensor_copy", [pa_u], [pa_f], out=pa_f[:], in_=pa_u[:])
        V("tensor_copy", [pb_u], [pb_f], out=pb_f[:], in_=pb_u[:])
        yield
        i16v = i16f.ap[:].rearrange("p (h q) a -> p h q a", q=2)
        e4i = cand.ap[:].rearrange("p h (r a) -> p h r a", a=16)
        e4j = candw.ap[:].rearrange("p h (r a) -> p h r a", a=16)
        io4 = iota16[:].unsqueeze(1).unsqueeze(1).to_broadcast([128, 8, 16, 16])
        V("tensor_tensor", [iota16, pa_f], [cand], out=e4i, in0=io4,
          in1=pa_f[:].unsqueeze(3).to_broadcast([128, 8, 16, 16]), op=ALU.is_equal)
        V("tensor_tensor", [iota16, pb_f], [candw], out=e4j, in0=io4,
          in1=pb_f[:].unsqueeze(3).to_broadcast([128, 8, 16, 16]), op=ALU.is_equal)
        yield
        GP("tensor_tensor", [candw, i16f], [candw], out=e4j, in0=e4j,
           in1=i16v[:, :, 1, :].unsqueeze(2).to_broadcast([128, 8, 16, 16]), op=ALU.mult)
        V("tensor_tensor", [cand, i16f], [cand], out=e4i, in0=e4i,
          in1=i16v[:, :, 0, :].unsqueeze(2).to_broadcast([128, 8, 16, 16]), op=ALU.mult)
        V("tensor_reduce", [cand], [isel], out=isel[:], in_=e4i, axis=AX.X, op=ALU.add)
        yield
        V("tensor_reduce", [candw], [jsel], out=jsel[:], in_=e4j, axis=AX.X, op=ALU.add)
        yield
        for (src, dstk, b) in ((isel, kI, 7), (jsel, kJ, 7), (gate, kG, 7)):
            TR([src, ident], [BK[b]], BK[b][:, 0:128], src[:].rearrange("p h r -> p (h r)"), ident[:])
            S("copy", [BK[b]], [dstk], out=dstk[:, c0:c0 + 128], in_=BK[b][:, 0:128])
        yield

    def gbuild(t4lo, t4hi):
        for t4 in range(t4lo, t4hi):
            b = t4 % 4
            for q in range(4):
                t = t4 * 4 + q
                sl = t % 4
                V("tensor_scalar", [iotab, kI], [Pm[sl]], out=Pm[sl][:], in0=iotab[:], scalar1=kI[:, t:t + 1],
                  scalar2=None, op0=ALU.is_equal)
                V("tensor_scalar", [iotab, kJ, kG], [Qm[sl]], out=Qm[sl][:], in0=iotab[:], scalar1=kJ[:, t:t + 1],
                  scalar2=kG[:, t:t + 1], op0=ALU.is_equal, op1=ALU.mult)
                MM([Pm[sl], Qm[sl]], [BK[b]], BK[b][:, q * 128:(q + 1) * 128], lhsT=Qm[sl][:], rhs=Pm[sl][:])
            tg = t4 * 4
            S("copy", [BK[b]], [OV], out=G_v[:, :, tg:tg + 4], in_=BK[b][:, :].rearrange("p (t i) -> p i t", i=128))
            yield

    dbg_row = [0]
    for pc in range(8):
        load_win_piece(pc)
    prep_load(0)
    n_prep = 0
    cpt = -(-NEXP_CH // max(NPT, 1))
    hTp = [hT, hT2]

    def prefix_front(ti):
        sl_ = ti % 2
        P.dma("xt%d" % sl_, [], [xt[sl_]], xt[sl_][:], xp[ti * 128:(ti + 1) * 128, :])
        yield from norm_transpose(xt[sl_], A1, B1, hTp[sl_], 0, 4)

    def prefix_back(ti):
        yield from hgrn_state_and_scores(False, hTp[ti % 2])

    def prep_some(n):
        nonlocal_n = [0]
        for _ in range(n):
            if n_prep_box[0] < NEXP_CH:
                prep_chunk(n_prep_box[0])
                n_prep_box[0] += 1
            yield

    n_prep_box = [0]
    if NPT > 0:
        run(prefix_front(0))
    for ti in range(NPT):
        if ti + 1 < NPT:
            interleave(prefix_back(ti), prefix_front(ti + 1))
        else:
            run(prefix_back(ti))
        run(prep_some(cpt))
    n_prep = n_prep_box[0]
    while n_prep < NEXP_CH:
        prep_chunk(n_prep)
        n_prep += 1
    for pc in range(8, 16):
        load_win_piece(pc)
    V("tensor_scalar", [S32, smallin], [S32], out=S32[:], in0=S32[:], scalar1=flg, scalar2=None, op0=ALU.mult)
    GP("tensor_copy", [S32], [SbA], out=SbA[:], in_=S32[:])

    n_out = 0
    for blk in range(NB):
        if blk == 0:
            for tt in range(2):
                P.dma("xt%d" % tt, [], [xt[tt]], xt[tt][:], xs[tt * 128:(tt + 1) * 128, :])
        t0i = blk * 2
        run(mixer_tile(xt[0], x1[0], 0, t0i * 128))
        def chain(*gens):
            for g_ in gens:
                yield from g_

        def xprefetch():
            if blk + 1 < NB:
                for tt in range(2):
                    ti = (blk + 1) * 2 + tt
                    P.dma("xt%d" % tt, [], [xt[tt]], xt[tt][:], xs[ti * 128:(ti + 1) * 128, :])
            if dbg:
                for tt in range(2):
                    ti = blk * 2 + tt
                    P.dma("dbg", [x1[tt]], [], dbg_d[ti * 128:(ti + 1) * 128, :], x1[tt][:])
            return
            yield

        sched([
            ("m1", lambda: chain(mixer_tile(xt[1], x1[1], 1, (t0i + 1) * 128), xprefetch()), []),
            ("q0", lambda: peer_q(0), []),
            ("k0", lambda: peer_topk(0), ["q0"]),
            ("q1", lambda: peer_q(1), ["m1", "q0"]),
            ("k1", lambda: peer_topk(1), ["q1", "k0"]),
            ("g0", lambda: gbuild(0, 32), ["k0", "m1"]),
            ("g1", lambda: gbuild(32, 64), ["k1", "g0"]),
        ])
        def load_ut(i):
            sl = i % NU
            P.dma("UTs%d" % sl, [("ut", i)], [UTs[sl]], UTs[sl][:], uv_s[i][:, 0:1024])

        def load_vb(i):
            sl = i % NV
            P.dma("VBs%d" % sl, [("vb", i)], [VBs[sl]], VBs[sl][:], uv_s[i][:, 1024:2048], eng="scalar")

        def issue_A(i):
            sl = i % NU
            b = i % 3
            for dc in range(8):
                MM([UTs[sl], h2T_t[0], h2T_t[1]], [BK[b]], BK[b][:, 0:TB], lhsT=UTs[sl][:, dc * 128:(dc + 1) * 128], rhs=h2T[:, dc, :],
                   start=(dc == 0), stop=(dc == 7))
            ACT([BK[b]], [ga[b]], ga[b][:], BK[b][:, 0:TB], AF.Gelu_apprx_tanh)
            V("tensor_tensor", [ga[b], OVK[i // 4]], [Wt[b]], out=Wt[b][:], in0=ga[b][:], in1=G_v[:, i, :], op=ALU.mult)

        def issue_Y(i):
            sl = i % NV
            b = i % 3
            for ts_ in range(2):
                for hf in range(2):
                    yb = 4 + ts_ * 2 + hf
                    MM([Wt[b], VBs[sl]], [BK[yb]], BK[yb][:, :], lhsT=Wt[b][:, ts_ * 128:(ts_ + 1) * 128],
                       rhs=VBs[sl][:, hf * 512:(hf + 1) * 512], start=(i == 0), stop=(i == NEXP_CH - 1))

        for i0 in range(NU - 1):
            load_ut(i0)
            if i0 < NV - 1:
                load_vb(i0)
        issue_A(0); issue_A(1)
        for i in range(NEXP_CH):
            if i + NU - 1 < NEXP_CH:
                load_ut(i + NU - 1)
            if i + NV - 1 < NEXP_CH:
                load_vb(i + NV - 1)
            if i + 2 < NEXP_CH:
                issue_A(i + 2)
            issue_Y(i)
            if blk + 1 < NB:
                if i < 96 and i % 12 == 11:
                    load_win_piece(i // 12)
                elif i >= 96 and i % 4 == 3:
                    load_win_piece(8 + (i - 96) // 4)
        for tt in range(2):
            ti = blk * 2 + tt
            for hf in range(2):
                yb = 4 + tt * 2 + hf
                V("tensor_tensor", [x1[tt], BK[yb]], [x1[tt]], out=x1[tt][:, hf * 512:(hf + 1) * 512],
                  in0=x1[tt][:, hf * 512:(hf + 1) * 512], in1=BK[yb][:, :], op=ALU.add)
            ACT([x1[tt]], [junk, st4], junk[:], x1[tt][:], AF.Square, accum_out=st4[:, 0:1])
            rstd_from_ss(st4[:, 0:1], D, st4[:, 1:2], [st4], [st4])
            so = stg[n_out % 2]
            V("scalar_tensor_tensor", [x1[tt], st4, fgb], [so], out=so[:], in0=x1[tt][:], scalar=st4[:, 1:2], in1=fgb[:],
              op0=ALU.mult, op1=ALU.mult)
            P.dma("stg%d" % (n_out % 2), [so], [], out_d[ti * 128:(ti + 1) * 128, :], so[:])
            n_out += 1
    P.wait_all_dma("sync", ["stg0", "stg1", "dbg", "dbg2"])
    es.close()
    print("instructions:", P.n_ins, "waits:", P.n_wait, "per-engine:", P.cnt)
    return nc


def make_in_maps(inputs, n_cores, ntok, npre):
    f = lambda a: np.ascontiguousarray(np.asarray(a, dtype=np.float32))
    x = f(inputs["x"]); c = f(inputs["c"])
    Bn, Sn, _ = x.shape
    halves = Sn // ntok
    fm = lambda v: np.ascontiguousarray(v.reshape(-1, 128).T)
    bc = lambda v: np.ascontiguousarray(np.broadcast_to(v[None, :], (128, v.shape[0])))
    ada_b = f(inputs["ada_b"])[0]
    lbg = f(inputs["lb_gamma"])
    common = {
        "ada_w": f(inputs["ada_w"])[0],
        "ada_bT": fm(ada_b),
        "abg": np.ascontiguousarray(np.stack([bc(ada_b[2048:3072]), bc(ada_b[5120:6144])], axis=1)),
        "n1gT": fm(f(inputs["norm1_g"])[0]),
        "n2gT": fm(f(inputs["norm2_g"])[0]),
        "w_in": f(inputs["w_in"])[0],
        "w_out": f(inputs["w_out"])[0],
        "wq": f(inputs["peer_wq"])[0],
        "lbg": np.ascontiguousarray(lbg.reshape(2, 4, 128).transpose(2, 0, 1)),
        "vec512": np.ascontiguousarray(np.stack([bc(f(inputs["hgrn_norm_g"])[0]), bc(f(inputs["gmlp_ln_g"])[0]),
                                                 bc(f(inputs["gmlp_ln_b"])[0]), bc(f(inputs["gmlp_norm_g"])[0])], axis=1)),
        "fg_bc": bc(f(inputs["final_g"])),
        "spw": f(inputs["spatial_w"])[0],
        "spbT": np.ascontiguousarray(f(inputs["spatial_b"])[0].T),
        "keys": np.ascontiguousarray(f(inputs["peer_keys"])[0].reshape(16, 128, 128)),
        "pu": f(inputs["peer_u"])[0],
        "pv": f(inputs["peer_v"])[0],
    }
    maps = []
    for core in range(n_cores):
        b = core // halves
        hf = core % halves
        m = dict(common)
        m["xs"] = np.ascontiguousarray(x[b, hf * ntok:(hf + 1) * ntok])
        if npre > 0:
            m["xp"] = np.ascontiguousarray(x[b, 0:npre])
        else:
            m["xp"] = np.ascontiguousarray(x[b, 0:128])
        m["flag"] = np.full((128, 1), 1.0 if hf > 0 else 0.0, np.float32)
        m["cT"] = fm(c[b])
        maps.append(m)
    return maps


def kernel(**inputs):
    x = np.asarray(inputs["x"])
    Bn, Sn, Dm = x.shape
    n_cores = 8
    ntok = Bn * Sn // n_cores
    halves = Sn // ntok
    npre = ntok if halves > 1 else 0
    nc = build_program(ntok, npre)
    maps = make_in_maps(inputs, n_cores, ntok, npre)
    res = run_bass_kernel_spmd(nc, maps, core_ids=list(range(n_cores)))
    out = np.empty((Bn, Sn, Dm), np.float32)
    for core in range(n_cores):
        b = core // halves
        hf = core % halves
        out[b, hf * ntok:(hf + 1) * ntok] = res.results[core]["out"]
    return out
```
